# Optimizing a Trainium2 kernel written in Bass

```python
import jax, jax.numpy as jnp
from jax import lax
import numpy as np

D_MODEL = 1024
BATCH = 8
SEQ = 4096
DEPTH = 4

RET_HEADS = 4
RET_DK = 64
RET_DV = 64
RET_CHUNK = 128
DIL_HEADS = 6
DIL_DH = 64
DIL_PATTERNS = ((128, 1), (512, 4), (2048, 16))
MLA_HEADS = 6
MLA_Q_RANK = 384
MLA_KV_RANK = 256
MLA_NOPE = 64
MLA_ROPE = 32
MLA_V = 64
Q_BLOCK = 128
RET_IN = RET_HEADS * (2 * RET_DK + 2 * RET_DV)
DIL_IN = 3 * DIL_HEADS * DIL_DH
MLA_IN = MLA_Q_RANK + MLA_KV_RANK + MLA_ROPE
D_IN = RET_IN + DIL_IN + MLA_IN
MIX_OUT = RET_HEADS * RET_DV + DIL_HEADS * DIL_DH + MLA_HEADS * MLA_V
D_FF = 2816
N_EXPERTS = 8
TOP_K = 2
D_FF_EXPERT = 3584
ROPE_THETA = 10000.0
LN_EPS = 1e-5
RMS_EPS = 1e-6
DEEPNORM_ALPHA = (2.0 * DEPTH) ** 0.25
DEEPNORM_BETA = (8.0 * DEPTH) ** -0.25
N_DENSE = (DEPTH + 1) // 2
N_MOE = DEPTH // 2

kernel_name = 'hybrid_ret_dilated_mla_moe_deepnorm'


def layer_norm(x, g, b):
    xf = x.astype(jnp.float32)
    mu = jnp.mean(xf, -1, keepdims=True)
    var = jnp.mean(jnp.square(xf - mu), -1, keepdims=True)
    return ((xf - mu) * lax.rsqrt(var + LN_EPS) * g + b).astype(x.dtype)


def rms_norm(x, g):
    xf = x.astype(jnp.float32)
    return (xf * lax.rsqrt(jnp.mean(jnp.square(xf), -1, keepdims=True) + RMS_EPS) * g).astype(x.dtype)


def rope_tables(positions, dim):
    inv = ROPE_THETA ** (-jnp.arange(0, dim, 2, dtype=jnp.float32) / dim)
    ang = positions.astype(jnp.float32)[..., None] * inv
    return jnp.cos(ang), jnp.sin(ang)


def apply_rope(x, cos, sin):
    c = cos[:, :, None, :].astype(x.dtype)
    s = sin[:, :, None, :].astype(x.dtype)
    x1, x2 = jnp.split(x, 2, axis=-1)
    return jnp.concatenate([x1 * c - x2 * s, x2 * c + x1 * s], axis=-1)


def retention(q, k, v, g, gn_g):
    B, S, H, dk = q.shape
    dv = v.shape[-1]
    C = RET_CHUNK
    N = S // C
    dt = q.dtype
    log_g = jnp.log(1.0 - 2.0 ** (-5.0 - jnp.arange(H, dtype=jnp.float32)))
    idx = jnp.arange(C, dtype=jnp.float32)
    diff = idx[:, None] - idx[None, :]
    intra = jnp.where(diff >= 0, jnp.exp(jnp.maximum(diff, 0.0) * log_g[:, None, None]), 0.0).astype(dt)
    xi = jnp.exp((idx[:, None] + 1.0) * log_g[None, :]).astype(dt)
    zeta = jnp.exp((C - 1.0 - idx[:, None]) * log_g[None, :]).astype(dt)
    chunk_decay = jnp.exp(C * log_g).astype(dt)[None, :, None, None]
    qc = q.reshape(B, N, C, H, dk)
    kc = k.reshape(B, N, C, H, dk) * (dk ** -0.5)
    vc = v.reshape(B, N, C, H, dv)
    s = jnp.einsum('bnihd,bnjhd->bnhij', qc, kc) * intra
    inner = jnp.einsum('bnhij,bnjhe->bnihe', s, vc)
    kv = jnp.einsum('bnjhd,bnjhe->nbhde', kc * zeta[:, :, None], vc)

    def step(state, kv_n):
        return state * chunk_decay + kv_n, state

    _, prev = lax.scan(step, jnp.zeros((B, H, dk, dv), dt), kv)
    cross = jnp.einsum('bnihd,nbhde->bnihe', qc, prev) * xi[:, :, None]
    o = (inner + cross).reshape(B, S, H, dv).astype(jnp.float32)
    mu = jnp.mean(o, -1, keepdims=True)
    var = jnp.mean(jnp.square(o - mu), -1, keepdims=True)
    o = ((o - mu) * lax.rsqrt(var + LN_EPS)).reshape(B, S, H * dv) * gn_g
    return jax.nn.silu(g) * o.astype(dt)


def dilated_branch(q, k, v, window, dilation):
    B, S, H, dh = q.shape
    L = S // dilation
    W = window // dilation
    nb = -(-L // W)
    Lp = nb * W
    R = B * dilation

    def to_sub(x):
        x = x.reshape(B, L, dilation, H, dh).transpose(0, 2, 1, 3, 4).reshape(R, L, H, dh)
        return jnp.pad(x, ((0, 0), (0, Lp - L), (0, 0), (0, 0)))

    def banded(x):
        cur = x.reshape(R, nb, W, H, dh)
        prev = jnp.pad(cur, ((0, 0), (1, 0), (0, 0), (0, 0), (0, 0)))[:, :nb]
        return jnp.concatenate([prev, cur], axis=2)

    qb = to_sub(q).reshape(R, nb, W, H, dh)
    kb = banded(to_sub(k))
    vb = banded(to_sub(v))
    s = jnp.einsum('rnqhd,rnkhd->rnhqk', qb, kb).astype(jnp.float32) * (dh ** -0.5)
    qi = jnp.arange(W)[:, None]
    kj = jnp.arange(2 * W)[None, :]
    dist = W + qi - kj
    band = (dist >= 0) & (dist <= W)
    first = jnp.arange(nb)[:, None, None] > 0
    valid = band[None] & (first | (kj >= W)[None])
    s = jnp.where(valid[None, :, None], s, -jnp.inf)
    m = jnp.max(s, -1, keepdims=True)
    p = jnp.exp(s - m)
    l = jnp.sum(p, -1, keepdims=True)
    o = jnp.einsum('rnhqk,rnkhd->rnqhd', (p / l).astype(v.dtype), vb)
    lse = (m + jnp.log(l))[..., 0]
    o = o.reshape(R, Lp, H, dh)[:, :L]
    o = o.reshape(B, dilation, L, H, dh).transpose(0, 2, 1, 3, 4).reshape(B, S, H, dh)
    lse = jnp.moveaxis(lse, 2, 3).reshape(R, Lp, H)[:, :L]
    lse = lse.reshape(B, dilation, L, H).transpose(0, 2, 1, 3).reshape(B, S, H)
    return o, lse


def dilated_attention(q, k, v):
    outs, lses = [], []
    for window, dilation in DIL_PATTERNS:
        o, lse = dilated_branch(q, k, v, window, dilation)
        outs.append(o)
        lses.append(lse)
    wts = jax.nn.softmax(jnp.stack(lses, 0), axis=0).astype(q.dtype)
    return jnp.einsum('pbsh,pbshd->bshd', wts, jnp.stack(outs, 0))


def mla_attention(qn, qr, kn, kr, v):
    B, S, H, _ = qn.shape
    nb = S // Q_BLOCK
    scale = (MLA_NOPE + MLA_ROPE) ** -0.5
    kpos = jnp.arange(S)

    def blocks(x):
        return jnp.moveaxis(x.reshape(B, nb, Q_BLOCK, *x.shape[2:]), 1, 0)

    def one(args):
        qn_b, qr_b, start = args
        s = jnp.einsum('bqhd,bkhd->bhqk', qn_b, kn) + jnp.einsum('bqhd,bkd->bhqk', qr_b, kr)
        s = s.astype(jnp.float32) * scale
        qpos = start + jnp.arange(Q_BLOCK)
        s = jnp.where(kpos[None, :] <= qpos[:, None], s, -jnp.inf)
        p = jax.nn.softmax(s, axis=-1).astype(v.dtype)
        return jnp.einsum('bhqk,bkhd->bqhd', p, v)

    starts = jnp.arange(nb, dtype=jnp.int32) * Q_BLOCK
    out = lax.map(one, (blocks(qn), blocks(qr), starts))
    return jnp.moveaxis(out, 0, 1).reshape(B, S, H, v.shape[-1])


def hybrid_mixer(h, w_in, ret_gn_g, mla_qn_g, mla_kvn_g, w_uq, w_ukv, w_out, rope_ret, rope_dil, rope_mla):
    B, S, _ = h.shape
    z = h @ w_in
    za, zb, zc = jnp.split(z, [RET_IN, RET_IN + DIL_IN], axis=-1)
    nq = RET_HEADS * RET_DK
    nv = RET_HEADS * RET_DV
    rq, rk, rv, rg = jnp.split(za, [nq, 2 * nq, 2 * nq + nv], axis=-1)
    rq = apply_rope(rq.reshape(B, S, RET_HEADS, RET_DK), *rope_ret)
    rk = apply_rope(rk.reshape(B, S, RET_HEADS, RET_DK), *rope_ret)
    ya = retention(rq, rk, rv.reshape(B, S, RET_HEADS, RET_DV), rg, ret_gn_g)
    dq, dk, dv = [t.reshape(B, S, DIL_HEADS, DIL_DH) for t in jnp.split(zb, 3, axis=-1)]
    dq = apply_rope(dq, *rope_dil)
    dk = apply_rope(dk, *rope_dil)
    yb = dilated_attention(dq, dk, dv).reshape(B, S, DIL_HEADS * DIL_DH)
    cq, ckv, kr = jnp.split(zc, [MLA_Q_RANK, MLA_Q_RANK + MLA_KV_RANK], axis=-1)
    q = (rms_norm(cq, mla_qn_g) @ w_uq).reshape(B, S, MLA_HEADS, MLA_NOPE + MLA_ROPE)
    qn, qr = jnp.split(q, [MLA_NOPE], axis=-1)
    qr = apply_rope(qr, *rope_mla)
    kv = (rms_norm(ckv, mla_kvn_g) @ w_ukv).reshape(B, S, MLA_HEADS, MLA_NOPE + MLA_V)
    kn, mv = jnp.split(kv, [MLA_NOPE], axis=-1)
    kr = apply_rope(kr[:, :, None, :], *rope_mla)[:, :, 0]
    yc = mla_attention(qn, qr, kn, kr, mv).reshape(B, S, MLA_HEADS * MLA_V)
    return jnp.concatenate([ya, yb, yc], axis=-1) @ w_out


def swiglu(h, w1, w3, w2):
    return (jax.nn.silu(h @ w1) * (h @ w3)) @ w2


def moe_swiglu(h, w_router, w1, w3, w2):
    B, S, D = h.shape
    t = h.reshape(B * S, D)
    logits = (t @ w_router).astype(jnp.float32)
    top_v, top_i = lax.top_k(logits, TOP_K)
    gates = jax.nn.softmax(top_v, axis=-1)
    combine = jnp.sum(jax.nn.one_hot(top_i, N_EXPERTS, dtype=jnp.float32) * gates[..., None], axis=1).astype(t.dtype)
    out = jnp.zeros_like(t)
    for e in range(N_EXPERTS):
        out = out + combine[:, e:e + 1] * swiglu(t, w1[e], w3[e], w2[e])
    return out.reshape(B, S, D)


def setup_inputs(seed: int = 0) -> dict:
    key = jax.random.key(seed)
    ks = jax.random.split(key, 23)
    f32 = jnp.float32

    def nrm(k, shape, scale):
        return jax.random.normal(k, shape, f32) * scale

    def gain(k, shape):
        return 1.0 + 0.02 * jax.random.normal(k, shape, f32)

    L = DEPTH
    D = D_MODEL
    return {
        'x': nrm(ks[0], (BATCH, SEQ, D), 1.0),
        'c': nrm(ks[1], (BATCH, D), 1.0),
        'positions': jnp.tile(jnp.arange(SEQ, dtype=jnp.int32)[None, :], (BATCH, 1)),
        'w_in': nrm(ks[2], (L, D, D_IN), D ** -0.5),
        'ret_gn_g': gain(ks[3], (L, RET_HEADS * RET_DV)),
        'mla_qn_g': gain(ks[4], (L, MLA_Q_RANK)),
        'mla_kvn_g': gain(ks[5], (L, MLA_KV_RANK)),
        'w_uq': nrm(ks[6], (L, MLA_Q_RANK, MLA_HEADS * (MLA_NOPE + MLA_ROPE)), MLA_Q_RANK ** -0.5),
        'w_ukv': nrm(ks[7], (L, MLA_KV_RANK, MLA_HEADS * (MLA_NOPE + MLA_V)), MLA_KV_RANK ** -0.5),
        'w_out': nrm(ks[8], (L, MIX_OUT, D), DEEPNORM_BETA * MIX_OUT ** -0.5),
        'w_ada': nrm(ks[9], (L, D, 6 * D), 0.1 * D ** -0.5),
        'b_ada': nrm(ks[10], (L, 6 * D), 0.01),
        'ln1_g': gain(ks[11], (L, D)),
        'ln1_b': nrm(ks[12], (L, D), 0.01),
        'ln2_g': gain(ks[13], (L, D)),
        'ln2_b': nrm(ks[14], (L, D), 0.01),
        'w1_dense': nrm(ks[15], (N_DENSE, D, D_FF), D ** -0.5),
        'w3_dense': nrm(ks[16], (N_DENSE, D, D_FF), D ** -0.5),
        'w2_dense': nrm(ks[17], (N_DENSE, D_FF, D), DEEPNORM_BETA * D_FF ** -0.5),
        'w_router': nrm(ks[18], (N_MOE, D, N_EXPERTS), D ** -0.5),
        'w1_moe': nrm(ks[19], (N_MOE, N_EXPERTS, D, D_FF_EXPERT), D ** -0.5),
        'w3_moe': nrm(ks[20], (N_MOE, N_EXPERTS, D, D_FF_EXPERT), D ** -0.5),
        'w2_moe': nrm(ks[21], (N_MOE, N_EXPERTS, D_FF_EXPERT, D), DEEPNORM_BETA * D_FF_EXPERT ** -0.5),
    }


def reference(x, c, positions, w_in, ret_gn_g, mla_qn_g, mla_kvn_g, w_uq, w_ukv, w_out,
              w_ada, b_ada, ln1_g, ln1_b, ln2_g, ln2_b, w1_dense, w3_dense, w2_dense,
              w_router, w1_moe, w3_moe, w2_moe):
    rope_ret = rope_tables(positions, RET_DK)
    rope_dil = rope_tables(positions, DIL_DH)
    rope_mla = rope_tables(positions, MLA_ROPE)
    cond = jax.nn.silu(c)
    for l in range(DEPTH):
        mod = (cond @ w_ada[l] + b_ada[l])[:, None, :]
        sh1, sc1, g1, sh2, sc2, g2 = jnp.split(mod, 6, axis=-1)
        h = x * (1.0 + sc1) + sh1
        y = hybrid_mixer(h, w_in[l], ret_gn_g[l], mla_qn_g[l], mla_kvn_g[l], w_uq[l], w_ukv[l], w_out[l],
                         rope_ret, rope_dil, rope_mla)
        x = layer_norm(DEEPNORM_ALPHA * x + (1.0 + g1) * y, ln1_g[l], ln1_b[l])
        h = x * (1.0 + sc2) + sh2
        if l % 2 == 0:
            y = swiglu(h, w1_dense[l // 2], w3_dense[l // 2], w2_dense[l // 2])
        else:
            y = moe_swiglu(h, w_router[l // 2], w1_moe[l // 2], w3_moe[l // 2], w2_moe[l // 2])
        x = layer_norm(DEEPNORM_ALPHA * x + (1.0 + g2) * y, ln2_g[l], ln2_b[l])
    return x
```

```python
import contextlib
import numpy as np
import concourse.bass as bass
import concourse.mybir as mybir
from concourse.bass_utils import run_bass_kernel_spmd

F32, BF16, I32 = mybir.dt.float32, mybir.dt.bfloat16, mybir.dt.int32
AF = mybir.ActivationFunctionType
ALU = mybir.AluOpType
AX = mybir.AxisListType

S = 4096
D = 1024
NT = 32
NB = 8
DEPTH = 4
ALPHA = (2.0 * DEPTH) ** 0.25
LN_EPS = 1e-5
RMS_EPS = 1e-6
THETA = 10000.0
NFM, NPM, NTM = 1952, 1312, 896
WIN = NFM + NPM + NTM
C_ID, C_LE, C_GE, C_DM, C_XI, C_ZE, C_IF64, C_SG64, C_IF32, C_SG32, C_SEL, C_END = (
    0, 128, 256, 384, 896, 1408, 1412, 1413, 1414, 1415, 1416, 1480)


def host_consts():
    c = np.zeros((128, C_END), np.float64)
    p = np.arange(128)
    c[:, C_ID:C_ID + 128] = np.eye(128)
    jj = p[:, None]
    ii = p[None, :]
    c[:, C_LE:C_LE + 128] = (jj <= ii)
    c[:, C_GE:C_GE + 128] = (jj >= ii)
    for h in range(4):
        lg = np.log(1.0 - 2.0 ** (-5.0 - h))
        c[:, C_DM + 128 * h:C_DM + 128 * (h + 1)] = np.where(ii >= jj, np.exp(np.maximum(ii - jj, 0) * lg), 0.0) * (64 ** -0.5)
        c[:, C_XI + 128 * h:C_XI + 128 * (h + 1)] = np.exp((ii + 1.0) * lg)
        c[:, C_ZE + h] = np.exp((127.0 - p) * lg) * (64 ** -0.5)
    c[:, C_IF64] = THETA ** (-(2.0 * (p % 32)) / 64.0) / (2 * np.pi)
    c[:, C_SG64] = np.where((p % 64) < 32, -2 * np.pi, 2 * np.pi)
    q = (p - 64) % 32
    c[:, C_IF32] = THETA ** (-(2.0 * (q % 16)) / 32.0) / (2 * np.pi)
    c[:, C_SG32] = np.where(q < 16, -2 * np.pi, 2 * np.pi)
    c[64, C_SEL:C_SEL + 64] = 1.0
    return c.astype(np.float32)


class Buf:
    __slots__ = ("w", "r", "name")

    def __init__(self, name=""):
        self.w = {}
        self.r = {}
        self.name = name


class T:
    def __init__(self, h, name):
        self.h = h
        self.b = Buf(name)

    def __getitem__(self, idx):
        return self.h[idx]


class Eng:
    def __init__(self, name, e, sem):
        self.name, self.e, self.sem, self.cnt, self.seen = name, e, sem, 0, {}
        self.ring = []
        self.di = 0

    def need(self, sem, val):
        if val <= 0 or self.seen.get(id(sem), 0) >= val:
            return
        self.e.wait_ge(sem, val)
        self.seen[id(sem)] = val


class Ctx:
    def __init__(self, nc, es):
        self.nc = nc
        self.es = es
        self.sems = {}
        mk = lambda n: es.enter_context(nc.semaphore(n))
        self.PE = Eng("pe", nc.tensor, mk("s_pe"))
        self.ACT = Eng("act", nc.scalar, mk("s_act"))
        self.DVE = Eng("dve", nc.vector, mk("s_dve"))
        self.POOL = Eng("pool", nc.gpsimd, mk("s_pool"))
        self.SP = Eng("sp", nc.sync, None)
        for i in range(8):
            self.SP.ring.append([mk(f"r_sp{i}"), 0])
            self.POOL.ring.append([mk(f"r_pl{i}"), 0])
        self.engs = [self.PE, self.ACT, self.DVE, self.POOL]
        self.nins = 0

    def _b(self, x):
        return x.b if isinstance(x, T) else x

    def _haz(self, E, reads, writes, own):
        for b in reads:
            for k, (sem, v) in b.w.items():
                E.need(sem, v)
        for b in writes:
            for k, (sem, v) in b.r.items():
                if sem is not own:
                    E.need(sem, v)
            for k, (sem, v) in b.w.items():
                if sem is not own:
                    E.need(sem, v)

    def _rec(self, sem, val, reads, writes):
        for b in reads:
            b.r[id(sem)] = (sem, val)
        for b in writes:
            if b.r:
                b.r = {}
                b.w = {}
            b.w[id(sem)] = (sem, val)

    def op(self, E, fn, reads=(), writes=()):
        reads = [self._b(x) for x in reads]
        writes = [self._b(x) for x in writes]
        self._haz(E, reads, writes, E.sem)
        ins = fn()
        E.cnt += 1
        ins.then_inc(E.sem, 1)
        self._rec(E.sem, E.cnt, reads, writes)
        self.nins += 1

    def dma(self, Q, out, in_, reads=(), writes=(), **kw):
        reads = [self._b(x) for x in reads]
        writes = [self._b(x) for x in writes]
        slot = Q.ring[Q.di % len(Q.ring)]
        Q.di += 1
        Q.need(slot[0], slot[1])
        self._haz(Q, reads, writes, None)
        ins = Q.e.dma_start(out=out, in_=in_, **kw)
        slot[1] += 16
        ins.then_inc(slot[0], 16)
        self._rec(slot[0], slot[1], reads, writes)
        self.nins += 1

    def barrier(self):
        tg = [(E.sem, E.cnt) for E in self.engs]
        for Q in (self.SP, self.POOL):
            tg += [(s[0], s[1]) for s in Q.ring]
        for E in self.engs + [self.SP]:
            for sem, v in tg:
                E.need(sem, v)


def build(nlayers=DEPTH, debug=False, stop=None):
    nc = bass.Bass("TRN2", target_bir_lowering=False)
    dt = nc.dram_tensor
    inp = lambda n, s, d=F32: dt(n, s, d, kind="ExternalInput").ap()
    skind = "ExternalOutput" if debug else "Internal"
    scr = lambda n, s, d=BF16: dt(n, s, d, kind=skind).ap()
    x_in = inp("x", [S, D])
    cT_in = inp("cT", [128, 8])
    pos_in = inp("pos", [1, S], I32)
    consts_in = inp("consts", [128, C_END])
    w_in_all = inp("w_in_all", [DEPTH, D, WIN])
    gn_g = inp("ret_gn_g", [DEPTH, 256])
    qn_gT = inp("qn_gT", [DEPTH, 128, 3])
    kvn_gT = inp("kvn_gT", [DEPTH, 128, 2])
    w_uq_all = inp("w_uq_all", [DEPTH, 384, 768])
    w_ukv_r = inp("w_ukv_r", [DEPTH, 256, 768])
    w_out = inp("w_out", [DEPTH, D, D])
    w_ada = inp("w_ada", [DEPTH, D, 6 * D])
    b_ada = inp("b_ada", [DEPTH, 6 * D])
    ln_in = {k: inp(k, [DEPTH, D]) for k in ("ln1_g", "ln1_b", "ln2_g", "ln2_b")}
    w1d = inp("w1_dense", [2, D, 2816])
    w3d = inp("w3_dense", [2, D, 2816])
    w2d = inp("w2_dense", [2, 2816, D])
    w_router = inp("w_router", [2, D, 8])
    w1m = inp("w1_moe", [2, 8, D, 3584])
    w3m = inp("w3_moe", [2, 8, D, 3584])
    w2m = inp("w2_moe", [2, 8, 3584, D])
    out = dt("out", [S, D], F32, kind="ExternalOutput").ap()
    XS = scr("XS", [S, D], F32)
    X1 = scr("X1", [S, D], F32)
    RQ = scr("RQ", [256, S])
    RQX = scr("RQX", [256, S])
    RK = scr("RK", [256, S])
    RV = scr("RV", [S, 256])
    RG = scr("RG", [S, 256])
    DQ = scr("DQ", [384, S])
    DK = scr("DK", [384, S])
    DV = scr("DV", [S, 384])
    MQ = scr("MQ", [6, 96, S])
    MKN = scr("MKN", [384, S])
    KR = scr("KR", [32, S])
    MV = scr("MV", [S, 384])
    CT = scr("CT", [16, 64, S])
    TB = scr("TB", [4, 128, S], F32)
    XSb, X1b = Buf("XS"), Buf("X1")
    SCR = Buf("scr_mix")
    CTb = Buf("CT")
    TBb = Buf("TB")

    with contextlib.ExitStack() as es:
        K = Ctx(nc, es)
        PE, ACT, DVE, POOL, SP = K.PE, K.ACT, K.DVE, K.POOL, K.SP

        uid = [0]

        def sb(es_, name, shape, dtype):
            uid[0] += 1
            name = f"{name}_{uid[0]}"
            return T(es_.enter_context(nc.sbuf_tensor(name, shape, dtype)), name)

        def ps(es_, name, shape, dtype=F32):
            uid[0] += 1
            name = f"{name}_{uid[0]}"
            return T(es_.enter_context(nc.psum_tensor(name, shape, dtype)), name)

        def act(o, i, func, reads, writes, **kw):
            K.op(ACT, lambda: nc.scalar.activation(out=o, in_=i, func=func, **kw), reads, writes)

        def tt(E, o, a, b, op, reads, writes):
            K.op(E, lambda: E.e.tensor_tensor(out=o, in0=a, in1=b, op=op), reads, writes)

        def ts(E, o, a, s1, s2, op0, op1, reads, writes):
            if s2 is None:
                K.op(E, lambda: E.e.tensor_scalar(out=o, in0=a, scalar1=s1, scalar2=None, op0=op0), reads, writes)
            else:
                K.op(E, lambda: E.e.tensor_scalar(out=o, in0=a, scalar1=s1, scalar2=s2, op0=op0, op1=op1), reads, writes)

        def stt(o, a, sc, b, op0, op1, reads, writes):
            K.op(DVE, lambda: nc.vector.scalar_tensor_tensor(out=o, in0=a, scalar=sc, in1=b, op0=op0, op1=op1), reads, writes)

        def cp(E, o, i, reads, writes):
            K.op(E, lambda: E.e.tensor_copy(out=o, in_=i), reads, writes)

        def mm(o, lhsT, rhs, start, stop, reads, writes):
            K.op(PE, lambda: nc.tensor.matmul(o, lhsT=lhsT, rhs=rhs, start=start, stop=stop), reads, writes)

        def tr(o, i, ident, reads, writes):
            K.op(PE, lambda: nc.tensor.transpose(o, i, ident), reads, writes)

        def recip(o, i, reads, writes):
            K.op(DVE, lambda: nc.vector.reciprocal(out=o, in_=i), reads, writes)

        def rsum(o, i, reads, writes):
            K.op(DVE, lambda: nc.vector.tensor_reduce(out=o, in_=i, axis=AX.X, op=ALU.add), reads, writes)

        cst = sb(es, "cst", [128, C_END], F32)
        K.dma(SP, cst[:], consts_in, writes=[cst])
        ident_bf = sb(es, "ident_bf", [128, 128], BF16)
        band_bf = sb(es, "band_bf", [128, 256], BF16)
        ones_f = sb(es, "ones_f", [128, 128], F32)
        ones_bf = sb(es, "ones_bf", [128, 128], BF16)
        eps_ln = sb(es, "eps_ln", [128, 1], F32)
        eps_rms = sb(es, "eps_rms", [128, 1], F32)
        cp(DVE, ident_bf[:], cst[:, C_ID:C_ID + 128], [cst], [ident_bf])
        cp(DVE, band_bf[:], cst[:, C_LE:C_LE + 256], [cst], [band_bf])
        K.op(DVE, lambda: nc.vector.memset(ones_f[:], 1.0), [], [ones_f])
        K.op(DVE, lambda: nc.vector.memset(ones_bf[:], 1.0), [], [ones_bf])
        K.op(DVE, lambda: nc.vector.memset(eps_ln[:], LN_EPS), [], [eps_ln])
        K.op(DVE, lambda: nc.vector.memset(eps_rms[:], RMS_EPS), [], [eps_rms])
        ident_f = cst
        silu_cT = sb(es, "silu_cT", [128, 8], F32)
        K.dma(SP, silu_cT[:], cT_in, writes=[silu_cT])
        act(silu_cT[:], silu_cT[:], AF.Silu, [silu_cT], [silu_cT])
        modcol = sb(es, "modcol", [128, 32], F32)
        g1p_bc = sb(es, "g1p_bc", [128, D], F32)
        g2p_bc = sb(es, "g2p_bc", [128, D], F32)
        lnp = {k: sb(es, "bc_" + k, [128, D], F32) for k in ln_in}

        with contextlib.ExitStack() as e1:
            posi = sb(e1, "posi", [128, S], I32)
            yv = sb(e1, "yv", [128, S], F32)
            ki = sb(e1, "ki", [128, S], I32)
            fr = sb(e1, "fr", [128, S], F32)
            m_ = sb(e1, "m_", [128, S], F32)
            K.dma(SP, posi[:], pos_in.broadcast_to([128, S]), writes=[posi])
            for ti, (cif, csg, shift) in enumerate(((C_IF64, None, 0.25), (C_IF64, C_SG64, 0.0), (C_IF32, None, 0.25), (C_IF32, C_SG32, 0.0))):
                cp(DVE, fr[:], posi[:], [posi], [fr])
                ts(DVE, yv[:], fr[:], cst[:, cif:cif + 1], shift, ALU.mult, ALU.add, [fr, cst], [yv])
                cp(DVE, ki[:], yv[:], [yv], [ki])
                cp(DVE, fr[:], ki[:], [ki], [fr])
                tt(DVE, fr[:], yv[:], fr[:], ALU.subtract, [yv, fr], [fr])
                ts(DVE, m_[:], fr[:], 0.5, None, ALU.is_gt, None, [fr], [m_])
                tt(DVE, fr[:], fr[:], m_[:], ALU.subtract, [fr, m_], [fr])
                ts(DVE, m_[:], fr[:], -0.5, None, ALU.is_lt, None, [fr], [m_])
                tt(DVE, fr[:], fr[:], m_[:], ALU.add, [fr, m_], [fr])
                if csg is None:
                    act(yv[:], fr[:], AF.Sin, [fr], [yv], scale=float(2 * np.pi))
                else:
                    act(yv[:], fr[:], AF.Sin, [fr, cst], [yv], scale=cst[:, csg:csg + 1])
                K.dma(SP, TB[ti], yv[:], reads=[yv], writes=[TBb])
            K.barrier()

        def load_hT(es_, xsrc, xbuf, blk, hT, col_sc, col_sh, xt, pst, hT32=None):
            for j in range(4):
                t = blk * 4 + j
                xx = xt[j % 2]
                K.dma(SP, xx[:], xsrc[t * 128:(t + 1) * 128, :], reads=[xbuf], writes=[xx])
                for k in range(8):
                    tr(pst[k][:, j * 128:(j + 1) * 128], xx[:, k * 128:(k + 1) * 128], cst[:, C_ID:C_ID + 128], [xx, cst], [pst[k]])
            for k in range(8):
                act(hT[:, k, :], pst[k][:], AF.Identity, [pst[k], modcol], [hT],
                    scale=modcol[:, col_sc + k:col_sc + k + 1], bias=modcol[:, col_sh + k:col_sh + k + 1])
                if hT32 is not None:
                    act(hT32[:, k, :], pst[k][:], AF.Identity, [pst[k], modcol], [hT32],
                        scale=modcol[:, col_sc + k:col_sc + k + 1], bias=modcol[:, col_sh + k:col_sh + k + 1])

        def layer_norm_store(u, gk, bk, dst, dstbuf, t, tmp, st):
            rsum(st[:, 0:1], u[:], [u], [st])
            act(tmp[:], u[:], AF.Square, [u], [tmp])
            rsum(st[:, 1:2], tmp[:], [tmp], [st])
            ts(DVE, st[:, 2:3], st[:, 0:1], 1.0 / D, None, ALU.mult, None, [st], [st])
            tt(DVE, st[:, 3:4], st[:, 2:3], st[:, 2:3], ALU.mult, [st], [st])
            stt(st[:, 4:5], st[:, 1:2], 1.0 / D, st[:, 3:4], ALU.mult, ALU.subtract, [st], [st])
            act(st[:, 5:6], st[:, 4:5], AF.Sqrt, [st, eps_ln], [st], bias=eps_ln[:, 0:1], scale=1.0)
            recip(st[:, 6:7], st[:, 5:6], [st], [st])
            stt(st[:, 7:8], st[:, 2:3], -1.0, st[:, 6:7], ALU.mult, ALU.mult, [st], [st])
            act(tmp[:], u[:], AF.Identity, [u, st], [tmp], scale=st[:, 6:7], bias=st[:, 7:8])
            tt(POOL, tmp[:], tmp[:], lnp[gk][:], ALU.mult, [tmp, lnp[gk]], [tmp])
            tt(POOL, u[:], tmp[:], lnp[bk][:], ALU.add, [tmp, lnp[bk]], [u])
            K.dma(SP, dst[t * 128:(t + 1) * 128, :], u[:], reads=[u], writes=[dstbuf])

        for l in range(nlayers):
            xsrc = x_in if l == 0 else XS
            xdst = out if l == nlayers - 1 else XS
            with contextlib.ExitStack() as e1:
                wad = [sb(e1, f"wad{i}", [128, 8, 512], F32) for i in range(2)]
                modrow = sb(e1, "modrow", [1, 6 * D], F32)
                brow = sb(e1, "brow", [1, 6 * D], F32)
                pm_ = [ps(e1, f"pm{i}", [128, 512]) for i in range(2)]
                pcol = ps(e1, "pcol", [128, 32])
                K.dma(SP, brow[:], b_ada[l:l + 1, :], writes=[brow])
                for k in lnp:
                    K.dma(SP, lnp[k][:], ln_in[k][l:l + 1, :].broadcast_to([128, D]), writes=[lnp[k]])
                for cb in range(12):
                    wv = wad[cb % 2]
                    K.dma(SP, wv[:], w_ada[l, :, cb * 512:(cb + 1) * 512].rearrange("(k p) n -> p k n", p=128), writes=[wv])
                    pp = pm_[cb % 2]
                    for k in range(8):
                        mm(pp[0:1, :], silu_cT[:, k:k + 1], wv[:, k, :], k == 0, k == 7, [silu_cT, wv], [pp])
                    tt(DVE, modrow[0:1, cb * 512:(cb + 1) * 512], pp[0:1, :], brow[0:1, cb * 512:(cb + 1) * 512], ALU.add,
                       [pp, brow], [modrow])
                for gi, (base, dst) in enumerate(((2 * D, g1p_bc), (5 * D, g2p_bc))):
                    for hf in range(2):
                        pp = pm_[hf]
                        mm(pp[:, :], ones_f[0:1, :], modrow[0:1, base + hf * 512: base + (hf + 1) * 512], True, True, [ones_f, modrow], [pp])
                        ts(DVE, dst[:, hf * 512:(hf + 1) * 512], pp[:], 1.0, None, ALU.add, None, [pp], [dst])
                for ci, base in enumerate((0, D, 3 * D, 4 * D)):
                    for k in range(8):
                        mm(pcol[:, ci * 8 + k: ci * 8 + k + 1], modrow[0:1, base + k * 128: base + (k + 1) * 128], ones_f[0:1, 0:1],
                           True, True, [modrow, ones_f], [pcol])
                cp(DVE, modcol[:], pcol[:], [pcol], [modcol])
                ts(DVE, modcol[:, 8:16], modcol[:, 8:16], 1.0, None, ALU.add, None, [modcol], [modcol])
                ts(DVE, modcol[:, 24:32], modcol[:, 24:32], 1.0, None, ALU.add, None, [modcol], [modcol])
                K.barrier()
            if stop == "M":
                break

            with contextlib.ExitStack() as e1:
                win = sb(e1, "win", [128, 8, WIN], BF16)
                for k in range(8):
                    K.dma(POOL, win[:, k, :], w_in_all[l, k * 128:(k + 1) * 128, :], writes=[win], max_dma_last_dim=4096)
                wuq = sb(e1, "wuq", [128, 3, 768], BF16)
                wukv = sb(e1, "wukv", [128, 2, 768], BF16)
                wq32 = sb(e1, "wq32", [128, 3, 768], F32)
                wkv32 = sb(e1, "wkv32", [128, 2, 768], F32)
                qng = sb(e1, "qng", [128, 3], F32)
                kvg = sb(e1, "kvg", [128, 2], F32)
                K.dma(SP, wq32[:], w_uq_all[l].rearrange("(k p) n -> p k n", p=128), writes=[wq32])
                K.dma(SP, wkv32[:], w_ukv_r[l].rearrange("(k p) n -> p k n", p=128), writes=[wkv32])
                K.dma(SP, qng[:], qn_gT[l], writes=[qng])
                K.dma(SP, kvg[:], kvn_gT[l], writes=[kvg])
                for k in range(3):
                    ts(DVE, wuq[:, k, :], wq32[:, k, :], qng[:, k:k + 1], None, ALU.mult, None, [wq32, qng], [wuq])
                for k in range(2):
                    ts(DVE, wukv[:, k, :], wkv32[:, k, :], kvg[:, k:k + 1], None, ALU.mult, None, [wkv32, kvg], [wukv])
                hT = sb(e1, "hT", [128, 8, 512], BF16)
                xt = [sb(e1, f"xt{i}", [128, D], F32) for i in range(2)]
                tb = sb(e1, "tb", [128, 4, 512], F32)
                fm = sb(e1, "fm", [128, 12, 512], BF16)
                tmo = sb(e1, "tmo", [128, 4, 896], BF16)
                t1 = sb(e1, "t1", [128, 512], F32)
                t2 = sb(e1, "t2", [128, 512], F32)
                cqT = sb(e1, "cqT", [128, 5, 512], BF16)
                sq = sb(e1, "sq", [128, 5, 512], F32)
                rs_q = sb(e1, "rs_q", [128, 512], F32)
                rs_kv = sb(e1, "rs_kv", [128, 512], F32)
                rs_col = sb(e1, "rs_col", [128, 4], F32)
                mqs = sb(e1, "mqs", [96, 6, 512], BF16)
                mkn = sb(e1, "mkn", [128, 3, 512], BF16)
                krs = sb(e1, "krs", [96, 512], BF16)
                mvs = sb(e1, "mvs", [128, 4, 384], BF16)
                with contextlib.ExitStack() as e2:
                    pst = [ps(e2, f"pst{i}", [128, 512]) for i in range(8)]
                    for blk in range(NB):
                        c0, c1 = blk * 512, (blk + 1) * 512
                        K.dma(SP, tb[:], TB[:, :, c0:c1].rearrange("a p t -> p a t"), reads=[TBb], writes=[tb])
                        load_hT(e2, xsrc, XSb, blk, hT, 8, 0, xt, pst)
                        bank = [0]

                        def nb():
                            bank[0] = (bank[0] + 1) % 8
                            return pst[bank[0]]
                        fm_off = [0, 128, 256, 384, 512, 640, 768, 896, 1024, 1152]
                        for ci in range(10):
                            pa, pb = nb(), nb()
                            fo = fm_off[ci]
                            for k in range(8):
                                mm(pa[:], win[:, k, fo:fo + 128], hT[:, k, :], k == 0, k == 7, [win, hT], [pa])
                            for k in range(8):
                                mm(pb[:], win[:, k, NFM + fo:NFM + fo + 128], hT[:, k, :], k == 0, k == 7, [win, hT], [pb])
                            tt(DVE, t1[:], pa[:], tb[:, 0, :], ALU.mult, [pa, tb], [t1])
                            tt(DVE, t2[:], pb[:], tb[:, 1, :], ALU.mult, [pb, tb], [t2])
                            slot = ci if ci < 2 else ci + 2
                            tt(DVE, fm[:, slot, :], t1[:], t2[:], ALU.add, [t1, t2], [fm])
                            if ci < 2:
                                for hh in range(2):
                                    h = ci * 2 + hh
                                    tt(POOL, fm[hh * 64:(hh + 1) * 64, 2 + ci, :].rearrange("p (c i) -> p c i", i=128),
                                       fm[hh * 64:(hh + 1) * 64, slot, :].rearrange("p (c i) -> p c i", i=128),
                                       cst[hh * 64:(hh + 1) * 64, C_XI + 128 * h:C_XI + 128 * (h + 1)].unsqueeze(1).broadcast_to([64, 4, 128]),
                                       ALU.mult, [fm, cst], [fm])
                        for ci in range(5):
                            pa = nb()
                            fo = 1280 + ci * 128
                            for k in range(8):
                                mm(pa[:], win[:, k, fo:fo + 128], hT[:, k, :], k == 0, k == 7, [win, hT], [pa])
                            act(cqT[:, ci, :], pa[:], AF.Copy, [pa], [cqT])
                            act(sq[:, ci, :], pa[:], AF.Square, [pa], [sq])
                        for (lo, n, rs, dim) in ((0, 3, rs_q, 384.0), (3, 2, rs_kv, 256.0)):
                            pa = nb()
                            for k in range(n):
                                mm(pa[:], ones_f[:], sq[:, lo + k, :], k == 0, k == n - 1, [ones_f, sq], [pa])
                            act(rs[:], pa[:], AF.Sqrt, [pa, eps_rms], [rs], scale=1.0 / dim, bias=eps_rms[:, 0:1])
                            recip(rs[:], rs[:], [rs], [rs])
                        pa, pb = nb(), nb()
                        for k in range(8):
                            mm(pa[64:96, :], win[:, k, 1920:1952], hT[:, k, :], k == 0, k == 7, [win, hT], [pa])
                        for k in range(8):
                            mm(pb[64:96, :], win[:, k, NFM + 1280:NFM + 1312], hT[:, k, :], k == 0, k == 7, [win, hT], [pb])
                        tt(DVE, t1[64:96, :], pa[64:96, :], tb[64:96, 2, :], ALU.mult, [pa, tb], [t1])
                        tt(DVE, t2[64:96, :], pb[64:96, :], tb[64:96, 3, :], ALU.mult, [pb, tb], [t2])
                        tt(DVE, krs[64:96, :], t1[64:96, :], t2[64:96, :], ALU.add, [t1, t2], [krs])
                        for h in range(6):
                            pa, pb = nb(), nb()
                            for k in range(3):
                                mm(pa[0:96, :], wuq[:, k, 96 * h:96 * h + 96], cqT[:, k, :], k == 0, k == 2, [wuq, cqT], [pa])
                            for k in range(3):
                                mm(pb[64:96, :], wuq[:, k, 576 + 32 * h:576 + 32 * h + 32], cqT[:, k, :], k == 0, k == 2, [wuq, cqT], [pb])
                            tt(DVE, mqs[0:64, h, :], pa[0:64, :], rs_q[0:64, :], ALU.mult, [pa, rs_q], [mqs])
                            tt(DVE, t1[64:96, :], pa[64:96, :], tb[64:96, 2, :], ALU.mult, [pa, tb], [t1])
                            tt(DVE, t2[64:96, :], pb[64:96, :], tb[64:96, 3, :], ALU.mult, [pb, tb], [t2])
                            tt(DVE, t1[64:96, :], t1[64:96, :], t2[64:96, :], ALU.add, [t1, t2], [t1])
                            tt(DVE, mqs[64:96, h, :], t1[64:96, :], rs_q[64:96, :], ALU.mult, [t1, rs_q], [mqs])
                        for c in range(3):
                            pa = nb()
                            for k in range(2):
                                mm(pa[:], wukv[:, k, c * 128:(c + 1) * 128], cqT[:, 3 + k, :], k == 0, k == 1, [wukv, cqT], [pa])
                            tt(DVE, mkn[:, c, :], pa[:], rs_kv[:], ALU.mult, [pa, rs_kv], [mkn])
                        for j in range(4):
                            tsl = slice(j * 128, (j + 1) * 128)
                            pa, pb, pc, pd = nb(), nb(), nb(), nb()
                            for k in range(8):
                                mm(pa[:], hT[:, k, tsl], win[:, k, NFM + NPM:NFM + NPM + 512], k == 0, k == 7, [win, hT], [pa])
                            for k in range(8):
                                mm(pb[:, 0:384], hT[:, k, tsl], win[:, k, NFM + NPM + 512:NFM + NPM + 896], k == 0, k == 7, [win, hT], [pb])
                            for k in range(2):
                                mm(pc[:, 0:384], cqT[:, 3 + k, tsl], wukv[:, k, 384:768], k == 0, k == 1, [wukv, cqT], [pc])
                            for k in range(2):
                                mm(pd[:, 0:1], sq[:, 3 + k, tsl], ones_f[:, 0:1], k == 0, k == 1, [sq, ones_f], [pd])
                            act(tmo[:, j, 0:256], pa[:, 0:256], AF.Copy, [pa], [tmo])
                            act(tmo[:, j, 256:512], pa[:, 256:512], AF.Silu, [pa], [tmo])
                            act(tmo[:, j, 512:896], pb[:, 0:384], AF.Copy, [pb], [tmo])
                            act(rs_col[:, j:j + 1], pd[:, 0:1], AF.Sqrt, [pd, eps_rms], [rs_col], scale=1.0 / 256.0, bias=eps_rms[:, 0:1])
                            recip(rs_col[:, j:j + 1], rs_col[:, j:j + 1], [rs_col], [rs_col])
                            ts(DVE, mvs[:, j, :], pc[:, 0:384], rs_col[:, j:j + 1], None, ALU.mult, None, [pc, rs_col], [mvs])
                        def fmv(dst, n):
                            return dst.rearrange("(c p) t -> p c t", p=128)[:, :, c0:c1]
                        K.dma(SP, fmv(RQ, 2), fm[:, 0:2, :], reads=[fm], writes=[SCR])
                        K.dma(SP, fmv(RQX, 2), fm[:, 2:4, :], reads=[fm], writes=[SCR])
                        K.dma(SP, fmv(RK, 2), fm[:, 4:6, :], reads=[fm], writes=[SCR])
                        K.dma(SP, fmv(DQ, 3), fm[:, 6:9, :], reads=[fm], writes=[SCR])
                        K.dma(SP, fmv(DK, 3), fm[:, 9:12, :], reads=[fm], writes=[SCR])
                        K.dma(SP, MQ[:, :, c0:c1].rearrange("h d t -> d h t"), mqs[:], reads=[mqs], writes=[SCR])
                        K.dma(SP, fmv(MKN, 3), mkn[:], reads=[mkn], writes=[SCR])
                        K.dma(SP, KR[:, c0:c1], krs[64:96, :], reads=[krs], writes=[SCR])
                        tmv = lambda dst: dst[c0:c1, :].rearrange("(j p) e -> p j e", p=128)
                        K.dma(SP, tmv(RV), tmo[:, :, 0:256], reads=[tmo], writes=[SCR])
                        K.dma(SP, tmv(RG), tmo[:, :, 256:512], reads=[tmo], writes=[SCR])
                        K.dma(SP, tmv(DV), tmo[:, :, 512:896], reads=[tmo], writes=[SCR])
                        K.dma(SP, tmv(MV), mvs[:], reads=[mvs], writes=[SCR])
                K.barrier()
            if stop == "A":
                break

            with contextlib.ExitStack() as e1:
                qT = sb(e1, "r_qT", [64, S], BF16)
                qxT = sb(e1, "r_qxT", [64, S], BF16)
                kT = sb(e1, "r_kT", [64, S], BF16)
                vv = sb(e1, "r_v", [128, NT, 64], BF16)
                gg = sb(e1, "r_g", [128, NT, 64], BF16)
                kz = sb(e1, "r_kz", [128, NT, 64], BF16)
                prev = sb(e1, "r_prev", [64, NT, 64], BF16)
                state = sb(e1, "r_state", [64, 64], F32)
                sTm = [sb(e1, f"r_sTm{i}", [128, 4, 128], BF16) for i in range(2)]
                osb = sb(e1, "r_osb", [128, 8, 64], F32)
                osq = sb(e1, "r_osq", [128, 8, 64], F32)
                obf = sb(e1, "r_obf", [128, 8, 64], BF16)
                st = sb(e1, "r_st", [128, 6, 8], F32)
                gng = sb(e1, "r_gng", [128, 256], F32)
                ost = sb(e1, "r_ost", [64, 1024], BF16)
                ptr = ps(e1, "r_ptr", [128, 1024], BF16)
                pkv = [ps(e1, f"r_pkv{i}", [64, 512]) for i in range(1)]
                pss = [ps(e1, f"r_pss{i}", [128, 512]) for i in range(2)]
                pso = ps(e1, "r_pso", [128, 512])
                pot = ps(e1, "r_pot", [64, 1024], BF16)
                K.dma(SP, gng[:], gn_g[l:l + 1, :].broadcast_to([128, 256]), writes=[gng])
                for h in range(4):
                    lg = float(np.log(1.0 - 2.0 ** (-5.0 - h)))
                    gC = float(np.exp(128.0 * lg))
                    hs = slice(h * 64, (h + 1) * 64)
                    K.dma(SP, qT[:], RQ[hs, :], reads=[SCR], writes=[qT])
                    K.dma(SP, qxT[:], RQX[hs, :], reads=[SCR], writes=[qxT])
                    K.dma(SP, kT[:], RK[hs, :], reads=[SCR], writes=[kT])
                    K.dma(SP, vv[:], RV[:, hs].rearrange("(t p) e -> p t e", p=128), reads=[SCR], writes=[vv])
                    K.dma(SP, gg[:], RG[:, hs].rearrange("(t p) e -> p t e", p=128), reads=[SCR], writes=[gg])
                    for g8 in range(4):
                        for c in range(8):
                            n = g8 * 8 + c
                            tr(ptr[:, c * 64:(c + 1) * 64], kT[:, n * 128:(n + 1) * 128], ident_bf[0:64, 0:64], [kT, ident_bf], [ptr])
                        act(kz[:, g8 * 8:(g8 + 1) * 8, :], ptr[:, 0:512].rearrange("p (c e) -> p c e", e=64), AF.Identity, [ptr, cst], [kz],
                            scale=cst[:, C_ZE + h:C_ZE + h + 1])
                    K.op(DVE, lambda: nc.vector.memset(state[:], 0.0), [], [state])
                    for g8 in range(4):
                        pk = pkv[0]
                        for c in range(8):
                            n = g8 * 8 + c
                            mm(pk[:, c * 64:(c + 1) * 64], kz[:, n, :], vv[:, n, :], True, True, [kz, vv], [pk])
                        for c in range(8):
                            n = g8 * 8 + c
                            cp(DVE, prev[:, n, :], state[:], [state], [prev])
                            stt(state[:], state[:], gC, pk[:, c * 64:(c + 1) * 64], ALU.mult, ALU.add, [state, pk], [state])
                    for g8 in range(4):
                        for half in range(2):
                            pS = pss[half]
                            for c in range(4):
                                n = g8 * 8 + half * 4 + c
                                cs = slice(n * 128, (n + 1) * 128)
                                mm(pS[:, c * 128:(c + 1) * 128], kT[:, cs], qT[:, cs], True, True, [kT, qT], [pS])
                            tt(DVE, sTm[half][:], pS[:].rearrange("p (c i) -> p c i", i=128),
                               cst[:, C_DM + 128 * h:C_DM + 128 * (h + 1)].unsqueeze(1).broadcast_to([128, 4, 128]),
                               ALU.mult, [pS, cst], [sTm[half]])
                            for c in range(4):
                                n = g8 * 8 + half * 4 + c
                                cc = half * 4 + c
                                cs = slice(n * 128, (n + 1) * 128)
                                mm(pso[:, cc * 64:(cc + 1) * 64], sTm[half][:, c, :], vv[:, n, :], True, False, [sTm[half], vv], [pso])
                                mm(pso[:, cc * 64:(cc + 1) * 64], qxT[:, cs], prev[:, n, :], False, True, [qxT, prev], [pso])
                        p3 = pso[:].rearrange("p (c e) -> p c e", e=64)
                        act(osb[:], p3, AF.Copy, [pso], [osb])
                        act(osq[:], p3, AF.Square, [pso], [osq])
                        rsum(st[:, 0, :], osb[:], [osb], [st])
                        rsum(st[:, 1, :], osq[:], [osq], [st])
                        ts(DVE, st[:, 2, :], st[:, 0, :], 1.0 / 64, None, ALU.mult, None, [st], [st])
                        tt(DVE, st[:, 3, :], st[:, 2, :], st[:, 2, :], ALU.mult, [st], [st])
                        stt(st[:, 4, :], st[:, 1, :], 1.0 / 64, st[:, 3, :], ALU.mult, ALU.subtract, [st], [st])
                        act(st[:, 5, :], st[:, 4, :], AF.Sqrt, [st, eps_ln], [st], bias=eps_ln[:, 0:1], scale=1.0)
                        recip(st[:, 5, :], st[:, 5, :], [st], [st])
                        tt(DVE, osb[:], osb[:], st[:, 2, :].unsqueeze(2).broadcast_to([128, 8, 64]), ALU.subtract, [osb, st], [osb])
                        tt(DVE, osb[:], osb[:], st[:, 5, :].unsqueeze(2).broadcast_to([128, 8, 64]), ALU.mult, [osb, st], [osb])
                        tt(DVE, osb[:], osb[:], gng[:, hs].unsqueeze(1).broadcast_to([128, 8, 64]), ALU.mult, [osb, gng], [osb])
                        tt(DVE, obf[:], osb[:], gg[:, g8 * 8:(g8 + 1) * 8, :], ALU.mult, [osb, gg], [obf])
                        for c in range(8):
                            tr(pot[:, c * 128:(c + 1) * 128], obf[:, c, :], ident_bf[:], [obf, ident_bf], [pot])
                        act(ost[:], pot[:], AF.Copy, [pot], [ost])
                        K.dma(SP, CT[h, :, g8 * 1024:(g8 + 1) * 1024], ost[:], reads=[ost], writes=[CTb])
                K.barrier()
            if stop == "R":
                break

            def normalize_store(e_, oacc, slot, nrm_ps, rcp, ostg):
                for qb in range(NB):
                    cs = slice(qb * 512, (qb + 1) * 512)
                    pn = nrm_ps[qb % 2]
                    mm(pn[0:64, :], cst[0:65, C_SEL:C_SEL + 64], oacc[0:65, cs], True, True, [cst, oacc], [pn])
                    recip(rcp[:, :], pn[0:64, :], [pn], [rcp])
                    tt(DVE, ostg[:, cs], oacc[0:64, cs], rcp[:, :], ALU.mult, [oacc, rcp], [ostg])
                K.dma(SP, CT[slot], ostg[:], reads=[ostg], writes=[CTb])

            with contextlib.ExitStack() as e1:
                qT = sb(e1, "d_qT", [64, S], BF16)
                kT = sb(e1, "d_kT", [64, S], BF16)
                vr = [sb(e1, f"d_v{i}", [128, NT, 65], BF16) for i in range(3)]
                oacc = sb(e1, "d_oacc", [65, S], F32)
                rcp = sb(e1, "d_rcp", [64, 512], F32)
                ostg = sb(e1, "d_ostg", [64, S], BF16)
                pT = [sb(e1, f"d_pT{i}", [128, 256], BF16) for i in range(3)]
                pss = [ps(e1, f"d_pss{i}", [128, 512]) for i in range(3)]
                pso = [ps(e1, f"d_pso{i}", [128, 512]) for i in range(2)]
                nrm = [ps(e1, f"d_nrm{i}", [128, 512]) for i in range(2)]
                for i in range(3):
                    K.op(POOL, lambda i=i: nc.gpsimd.memset(vr[i][:], 1.0), [], [vr[i]])
                it = 0
                for h in range(6):
                    hs = slice(h * 64, (h + 1) * 64)
                    K.dma(SP, qT[:], DQ[hs, :], reads=[SCR], writes=[qT])
                    K.dma(SP, kT[:], DK[hs, :], reads=[SCR], writes=[kT])
                    for pi, r in enumerate((1, 4, 16)):
                        nkt = NT // r
                        for rho in range(r):
                            src = DV[:, hs].rearrange("(kt j rr) e -> rr j kt e", j=128, rr=r)[rho]
                            K.dma(SP, vr[pi][:, rho * nkt:(rho + 1) * nkt, 0:64], src, reads=[SCR], writes=[vr[pi]])
                    for pi, r in enumerate((1, 4, 16)):
                        nkt = NT // r
                        L = S // r
                        qv = qT[:].rearrange("d (m rr) -> d rr m", rr=r)
                        kv_ = kT[:].rearrange("d (m rr) -> d rr m", rr=r)
                        ov = oacc[:].rearrange("d (m rr) -> d rr m", rr=r)
                        for rho in range(r):
                            for kt in range(nkt):
                                nq = 256 if kt < nkt - 1 else 128
                                pS = pss[it % 3]
                                pt = pT[it % 3]
                                it += 1
                                mm(pS[:, 0:nq], kv_[:, rho, kt * 128:(kt + 1) * 128], qv[:, rho, kt * 128:kt * 128 + nq], True, True, [kT, qT], [pS])
                                act(pt[:, 0:nq], pS[:, 0:nq], AF.Exp, [pS], [pt], scale=0.125)
                                tt(POOL, pt[:, 0:nq], pt[:, 0:nq], band_bf[:, 0:nq], ALU.mult, [pt, band_bf], [pt])
                                bk = kt // 4
                                po = pso[bk % 2]
                                vt = vr[pi][:, rho * nkt + kt, :]
                                c = kt % 4
                                mm(po[0:65, c * 128:(c + 1) * 128], vt, pt[:, 0:128], kt == 0, True, [vr[pi], pt], [po])
                                if nq == 256:
                                    bk2 = (kt + 1) // 4
                                    po2 = pso[bk2 % 2]
                                    c2 = (kt + 1) % 4
                                    mm(po2[0:65, c2 * 128:(c2 + 1) * 128], vt, pt[:, 128:256], True, False, [vr[pi], pt], [po2])
                                if c == 3 or kt == nkt - 1:
                                    w = (c + 1) * 128
                                    m0 = bk * 512
                                    if pi == 0:
                                        act(ov[0:65, rho, m0:m0 + w], po[0:65, 0:w], AF.Copy, [po], [oacc])
                                    else:
                                        tt(DVE, ov[0:65, rho, m0:m0 + w], po[0:65, 0:w], ov[0:65, rho, m0:m0 + w], ALU.add, [po, oacc], [oacc])
                    normalize_store(e1, oacc, 4 + h, nrm, rcp, ostg)
                K.barrier()
            if stop == "D":
                break

            with contextlib.ExitStack() as e1:
                qT = sb(e1, "m_qT", [96, S], BF16)
                kT = sb(e1, "m_kT", [96, S], BF16)
                vt_ = sb(e1, "m_v", [128, NT, 65], BF16)
                oacc = sb(e1, "m_oacc", [65, S], F32)
                rcp = sb(e1, "m_rcp", [64, 512], F32)
                ostg = sb(e1, "m_ostg", [64, S], BF16)
                pT = [sb(e1, f"m_pT{i}", [128, 512], BF16) for i in range(3)]
                pss = [ps(e1, f"m_pss{i}", [128, 512]) for i in range(3)]
                pso = [ps(e1, f"m_pso{i}", [128, 512]) for i in range(2)]
                nrm = [ps(e1, f"m_nrm{i}", [128, 512]) for i in range(2)]
                K.op(POOL, lambda: nc.gpsimd.memset(vt_[:], 1.0), [], [vt_])
                sc = float(96 ** -0.5)
                it = 0
                for h in range(6):
                    K.dma(SP, qT[:], MQ[h], reads=[SCR], writes=[qT])
                    K.dma(SP, kT[0:64, :], MKN[h * 64:(h + 1) * 64, :], reads=[SCR], writes=[kT])
                    K.dma(SP, kT[64:96, :], KR, reads=[SCR], writes=[kT])
                    K.dma(SP, vt_[:, :, 0:64], MV[:, h * 64:(h + 1) * 64].rearrange("(t p) e -> p t e", p=128), reads=[SCR], writes=[vt_])
                    for qb in range(NB):
                        po = pso[qb % 2]
                        nk = 4 * qb + 4
                        for kt in range(nk):
                            j = kt - 4 * qb
                            q0 = 0 if j < 0 else 128 * j
                            pS = pss[it % 3]
                            pt = pT[it % 3]
                            it += 1
                            mm(pS[:, q0:512], kT[:, kt * 128:(kt + 1) * 128], qT[:, qb * 512 + q0:(qb + 1) * 512], True, True, [kT, qT], [pS])
                            act(pt[:, q0:512], pS[:, q0:512], AF.Exp, [pS], [pt], scale=sc)
                            if j >= 0:
                                tt(POOL, pt[:, q0:q0 + 128], pt[:, q0:q0 + 128], band_bf[:, 0:128], ALU.mult, [pt, band_bf], [pt])
                            mm(po[0:65, q0:512], vt_[:, kt, :], pt[:, q0:512], kt == 0, kt == nk - 1, [vt_, pt], [po])
                        act(oacc[0:65, qb * 512:(qb + 1) * 512], po[0:65, :], AF.Copy, [po], [oacc])
                    normalize_store(e1, oacc, 10 + h, nrm, rcp, ostg)
                K.barrier()
            if stop == "ML":
                break

            with contextlib.ExitStack() as e1:
                wo32 = sb(e1, "wo32", [128, 8, D], F32)
                wo = sb(e1, "wo", [128, 8, D], BF16)
                K.dma(SP, wo32[:], w_out[l].rearrange("(c p) n -> p c n", p=128), writes=[wo32])
                for c in range(8):
                    tt(POOL if c % 2 else DVE, wo[:, c, :], wo32[:, c, :], g1p_bc[:], ALU.mult, [wo32, g1p_bc], [wo])
                ct = [sb(e1, f"o_ct{i}", [128, 8, 512], BF16) for i in range(2)]
                xt = [sb(e1, f"o_xt{i}", [128, D], F32) for i in range(2)]
                u = [sb(e1, f"o_u{i}", [128, D], F32) for i in range(2)]
                tmp = sb(e1, "o_tmp", [128, D], F32)
                st = sb(e1, "o_st", [128, 8], F32)
                py = [ps(e1, f"o_py{i}", [128, 512]) for i in range(4)]
                for blk in range(NB):
                    cb = ct[blk % 2]
                    K.dma(SP, cb[:], CT[:, :, blk * 512:(blk + 1) * 512].rearrange("(c s) d t -> (s d) c t", s=2), reads=[CTb], writes=[cb])
                    for j in range(4):
                        t = blk * 4 + j
                        xx = xt[t % 2]
                        uu = u[t % 2]
                        K.dma(SP, xx[:], xsrc[t * 128:(t + 1) * 128, :], reads=[XSb], writes=[xx])
                        for hf in range(2):
                            pp = py[(t % 2) * 2 + hf]
                            for c in range(8):
                                mm(pp[:], cb[:, c, j * 128:(j + 1) * 128], wo[:, c, hf * 512:(hf + 1) * 512], c == 0, c == 7, [cb, wo], [pp])
                            stt(uu[:, hf * 512:(hf + 1) * 512], xx[:, hf * 512:(hf + 1) * 512], ALPHA, pp[:], ALU.mult, ALU.add, [xx, pp], [uu])
                        layer_norm_store(uu, "ln1_g", "ln1_b", X1, X1b, t, tmp, st)
                K.barrier()
            if stop == "O":
                break

            moe = (l % 2 == 1)
            li = l // 2
            NE = 8 if moe else 1
            DFF = 3584 if moe else 2816
            NG = DFF // 256
            with contextlib.ExitStack() as e1:
                hT = sb(e1, "f_hT", [128, 8, 2048], BF16)
                yacc = sb(e1, "f_yacc", [128, 16, D], F32)
                gT = [sb(e1, f"f_gT{i}", [128, 2, 2048], BF16) for i in range(2)]
                wst = sb(e1, "f_wst", [128, 3, 2048], F32)
                wb = [sb(e1, f"f_wb{i}", [128, 3, 2048], BF16) for i in range(2)]
                xt = [sb(e1, f"f_xt{i}", [128, D], F32) for i in range(2)]
                sa = [sb(e1, f"f_sa{i}", [128, 512], BF16) for i in range(2)]
                comb = sb(e1, "f_comb", [128, 16, 8], F32)
                lg = sb(e1, "f_lg", [128, 8], F32)
                lt = sb(e1, "f_lt", [128, 8], F32)
                mk1 = sb(e1, "f_mk1", [128, 8], F32)
                mk2 = sb(e1, "f_mk2", [128, 8], F32)
                mx = sb(e1, "f_mx", [128, 4], F32)
                st = sb(e1, "f_st", [128, 8], F32)
                wr = sb(e1, "f_wr", [128, 8, 8], F32)
                pbank = [ps(e1, f"f_p{i}", [128, 512]) for i in range(8)]
                if moe:
                    K.dma(SP, wr[:], w_router[li].rearrange("(k p) e -> p k e", p=128), writes=[wr])
                for sbk in range(2):
                    for b4 in range(4):
                        blk = sbk * 4 + b4
                        for j in range(4):
                            t = blk * 4 + j
                            xx = xt[j % 2]
                            K.dma(SP, xx[:], X1[t * 128:(t + 1) * 128, :], reads=[X1b], writes=[xx])
                            for k in range(8):
                                tr(pbank[k][:, j * 128:(j + 1) * 128], xx[:, k * 128:(k + 1) * 128], cst[:, C_ID:C_ID + 128], [xx, cst], [pbank[k]])
                        for k in range(8):
                            act(hT[:, k, b4 * 512:(b4 + 1) * 512], pbank[k][:], AF.Identity, [pbank[k], modcol], [hT],
                                scale=modcol[:, 24 + k:25 + k], bias=modcol[:, 16 + k:17 + k])
                        if moe:
                            h32 = wst[:, 0:2, :].rearrange("p a (k t) -> p (a k) t", t=512)
                            for k in range(8):
                                act(h32[:, k, :], pbank[k][:], AF.Identity, [pbank[k], modcol], [wst],
                                    scale=modcol[:, 24 + k:25 + k], bias=modcol[:, 16 + k:17 + k])
                            for j in range(4):
                                tl = b4 * 4 + j
                                pr = pbank[j]
                                for k in range(8):
                                    mm(pr[:, 0:8], h32[:, k, j * 128:(j + 1) * 128], wr[:, k, :], k == 0, k == 7, [wst, wr], [pr])
                                cp(DVE, lg[:], pr[:, 0:8], [pr], [lg])
                                K.op(DVE, lambda: nc.vector.tensor_reduce(out=mx[:, 0:1], in_=lg[:], axis=AX.X, op=ALU.max), [lg], [mx])
                                ts(DVE, mk1[:], lg[:], mx[:, 0:1], None, ALU.is_ge, None, [lg, mx], [mk1])
                                stt(lt[:], mk1[:], -1e30, lg[:], ALU.mult, ALU.add, [mk1, lg], [lt])
                                K.op(DVE, lambda: nc.vector.tensor_reduce(out=mx[:, 1:2], in_=lt[:], axis=AX.X, op=ALU.max), [lt], [mx])
                                ts(DVE, mk2[:], lt[:], mx[:, 1:2], None, ALU.is_ge, None, [lt, mx], [mk2])
                                tt(DVE, mx[:, 2:3], mx[:, 1:2], mx[:, 0:1], ALU.subtract, [mx], [mx])
                                act(mx[:, 2:3], mx[:, 2:3], AF.Exp, [mx], [mx])
                                ts(DVE, mx[:, 3:4], mx[:, 2:3], 1.0, None, ALU.add, None, [mx], [mx])
                                recip(mx[:, 3:4], mx[:, 3:4], [mx], [mx])
                                tt(DVE, mx[:, 2:3], mx[:, 2:3], mx[:, 3:4], ALU.mult, [mx], [mx])
                                ts(DVE, mk1[:], mk1[:], mx[:, 3:4], None, ALU.mult, None, [mk1, mx], [mk1])
                                stt(comb[:, tl, :], mk2[:], mx[:, 2:3], mk1[:], ALU.mult, ALU.add, [mk2, mx, mk1], [comb])
                    gi = 0
                    for e in range(NE):
                        if moe:
                            W1, W3, W2 = w1m[li, e], w3m[li, e], w2m[li, e]
                        else:
                            W1, W3, W2 = w1d[li], w3d[li], w2d[li]
                        for g in range(NG):
                            f0 = g * 256
                            wbb = wb[gi % 2]
                            gTT = gT[gi % 2]
                            gi += 1
                            K.dma(SP, wst[:, 0, :].rearrange("p (k n) -> p k n", n=256), W1[:, f0:f0 + 256].rearrange("(k p) n -> p k n", p=128), writes=[wst])
                            K.dma(SP, wst[:, 1, :].rearrange("p (k n) -> p k n", n=256), W3[:, f0:f0 + 256].rearrange("(k p) n -> p k n", p=128), writes=[wst])
                            K.dma(SP, wst[:, 2, :].rearrange("p (k n) -> p k n", n=1024), W2[f0:f0 + 256, :].rearrange("(k p) n -> p k n", p=128), writes=[wst])
                            cp(POOL, wbb[:, 0, :], wst[:, 0, :], [wst], [wbb])
                            cp(ACT if False else POOL, wbb[:, 1, :], wst[:, 1, :], [wst], [wbb])
                            cp(DVE, wbb[:, 2, :], wst[:, 2, :], [wst], [wbb])
                            w1v = wbb[:, 0, :].rearrange("p (k n) -> p k n", n=256)
                            w3v = wbb[:, 1, :].rearrange("p (k n) -> p k n", n=256)
                            w2v = wbb[:, 2, :].rearrange("p (k n) -> p k n", n=1024)
                            bi = 0
                            for fc in range(2):
                                for b4 in range(4):
                                    pa = pbank[(bi % 2) * 2]
                                    pb = pbank[(bi % 2) * 2 + 1]
                                    sa_ = sa[bi % 2]
                                    bi += 1
                                    ts_ = slice(b4 * 512, (b4 + 1) * 512)
                                    for k in range(8):
                                        mm(pa[:], w1v[:, k, fc * 128:(fc + 1) * 128], hT[:, k, ts_], k == 0, k == 7, [wbb, hT], [pa])
                                    for k in range(8):
                                        mm(pb[:], w3v[:, k, fc * 128:(fc + 1) * 128], hT[:, k, ts_], k == 0, k == 7, [wbb, hT], [pb])
                                    act(sa_[:], pa[:], AF.Silu, [pa], [sa_])
                                    tt(DVE, gTT[:, fc, ts_], pb[:], sa_[:], ALU.mult, [pb, sa_], [gTT])
                            for tl in range(16):
                                for hf in range(2):
                                    pp = pbank[4 + ((tl * 2 + hf) % 4)]
                                    for fc in range(2):
                                        mm(pp[:], gTT[:, fc, tl * 128:(tl + 1) * 128], w2v[:, fc, hf * 512:(hf + 1) * 512], fc == 0, fc == 1, [gTT, wbb], [pp])
                                    ya = yacc[:, tl, hf * 512:(hf + 1) * 512]
                                    first = (e == 0 and g == 0)
                                    if moe:
                                        if first:
                                            ts(DVE, ya, pp[:], comb[:, tl, e:e + 1], None, ALU.mult, None, [pp, comb], [yacc])
                                        else:
                                            stt(ya, pp[:], comb[:, tl, e:e + 1], ya, ALU.mult, ALU.add, [pp, comb, yacc], [yacc])
                                    else:
                                        if first:
                                            cp(DVE, ya, pp[:], [pp], [yacc])
                                        else:
                                            tt(DVE, ya, pp[:], ya, ALU.add, [pp, yacc], [yacc])
                    for tl in range(16):
                        t = sbk * 16 + tl
                        xx = xt[tl % 2]
                        K.dma(SP, xx[:], X1[t * 128:(t + 1) * 128, :], reads=[X1b], writes=[xx])
                        yv_ = yacc[:, tl, :]
                        tt(POOL, yv_, yv_, g2p_bc[:], ALU.mult, [yacc, g2p_bc], [yacc])
                        stt(xx[:], xx[:], ALPHA, yv_, ALU.mult, ALU.add, [xx, yacc], [xx])
                        layer_norm_store(xx, "ln2_g", "ln2_b", xdst, XSb, t, _TV(wst), st)
                K.barrier()
        K.barrier()
    return nc


class _TV(T):
    def __init__(self, wst):
        self.h = wst.h
        self.b = wst.b

    def __getitem__(self, idx):
        return self.h[:, 0, 0:1024]


def prep_inputs(inputs):
    f = lambda a: np.ascontiguousarray(np.asarray(a))
    w_in = f(inputs["w_in"])
    L = w_in.shape[0]
    h64 = np.arange(64)
    p64 = (h64 + 32) % 64
    p32 = (np.arange(32) + 16) % 32
    rq = np.arange(0, 256); rk = np.arange(256, 512); rv = np.arange(512, 768); rg = np.arange(768, 1024)
    dq = np.arange(1024, 1408); dk = np.arange(1408, 1792); dv = np.arange(1792, 2176)
    cq = np.arange(2176, 2560); ckv = np.arange(2560, 2816); kr = np.arange(2816, 2848)

    def permheads(cols, nh):
        return np.concatenate([cols[h * 64:(h + 1) * 64][p64] for h in range(nh)])
    fm_cols = np.concatenate([rq, rk, dq, dk, cq, ckv, kr])
    pm_cols = np.concatenate([permheads(rq, 4), permheads(rk, 4), permheads(dq, 6), permheads(dk, 6), kr[p32]])
    tm_cols = np.concatenate([rv, rg, dv])
    allc = np.concatenate([fm_cols, pm_cols, tm_cols])
    assert allc.shape[0] == WIN
    w_in_all = np.ascontiguousarray(w_in[:, :, allc])
    w_uq = f(inputs["w_uq"])
    uq_pm = np.concatenate([96 * h + 64 + p32 for h in range(6)])
    w_uq_all = np.ascontiguousarray(np.concatenate([w_uq, w_uq[:, :, uq_pm]], axis=2))
    w_ukv = f(inputs["w_ukv"])
    kn_cols = np.concatenate([128 * h + np.arange(64) for h in range(6)])
    v_cols = np.concatenate([128 * h + 64 + np.arange(64) for h in range(6)])
    w_ukv_r = np.ascontiguousarray(w_ukv[:, :, np.concatenate([kn_cols, v_cols])])
    shared = {
        "consts": host_consts(),
        "w_in_all": w_in_all,
        "ret_gn_g": f(inputs["ret_gn_g"]),
        "qn_gT": np.ascontiguousarray(f(inputs["mla_qn_g"]).reshape(L, 3, 128).transpose(0, 2, 1)),
        "kvn_gT": np.ascontiguousarray(f(inputs["mla_kvn_g"]).reshape(L, 2, 128).transpose(0, 2, 1)),
        "w_uq_all": w_uq_all,
        "w_ukv_r": w_ukv_r,
    }
    for k in ("w_out", "w_ada", "b_ada", "ln1_g", "ln1_b", "ln2_g", "ln2_b", "w1_dense", "w3_dense", "w2_dense",
              "w_router", "w1_moe", "w3_moe", "w2_moe"):
        shared[k] = f(inputs[k])
    x = f(inputs["x"]); c = f(inputs["c"]); pos = f(inputs["positions"]).astype(np.int32)
    maps = []
    for b in range(x.shape[0]):
        m = dict(shared)
        m["x"] = x[b]
        m["cT"] = np.ascontiguousarray(c[b].reshape(8, 128).T)
        m["pos"] = pos[b:b + 1]
        maps.append(m)
    return maps


def kernel(**inputs):
    maps = prep_inputs(inputs)
    nc = build()
    res = run_bass_kernel_spmd(nc, maps, core_ids=list(range(8)))
    return np.stack([np.asarray(r["out"]) for r in res.results], axis=0).astype(np.float32)
```

```python
import contextlib
import numpy as np
import concourse.bass as bass
import concourse.mybir as mybir
from concourse.bass_utils import run_bass_kernel_spmd

F32, BF16, I32 = mybir.dt.float32, mybir.dt.bfloat16, mybir.dt.int32
AF = mybir.ActivationFunctionType
ALU = mybir.AluOpType
AX = mybir.AxisListType

S = 4096
D = 1024
NT = 32
NB = 8
DEPTH = 4
ALPHA = (2.0 * DEPTH) ** 0.25
LN_EPS = 1e-5
RMS_EPS = 1e-6
THETA = 10000.0
NFM, NPM, NTM = 1952, 1312, 896
WIN = NFM + NPM + NTM
C_ID, C_LE, C_GE, C_DM, C_XI, C_ZE, C_IF64, C_SG64, C_IF32, C_SG32, C_SEL, C_END = (
    0, 128, 256, 384, 896, 1408, 1412, 1413, 1414, 1415, 1416, 1480)


def host_consts():
    c = np.zeros((128, C_END), np.float64)
    p = np.arange(128)
    c[:, C_ID:C_ID + 128] = np.eye(128)
    jj = p[:, None]
    ii = p[None, :]
    c[:, C_LE:C_LE + 128] = (jj <= ii)
    c[:, C_GE:C_GE + 128] = (jj >= ii)
    for h in range(4):
        lg = np.log(1.0 - 2.0 ** (-5.0 - h))
        c[:, C_DM + 128 * h:C_DM + 128 * (h + 1)] = np.where(ii >= jj, np.exp(np.maximum(ii - jj, 0) * lg), 0.0) * (64 ** -0.5)
        c[:, C_XI + 128 * h:C_XI + 128 * (h + 1)] = np.exp((ii + 1.0) * lg)
        c[:, C_ZE + h] = np.exp((127.0 - p) * lg) * (64 ** -0.5)
    c[:, C_IF64] = THETA ** (-(2.0 * (p % 32)) / 64.0) / (2 * np.pi)
    c[:, C_SG64] = np.where((p % 64) < 32, -2 * np.pi, 2 * np.pi)
    q = (p - 64) % 32
    c[:, C_IF32] = THETA ** (-(2.0 * (q % 16)) / 32.0) / (2 * np.pi)
    c[:, C_SG32] = np.where(q < 16, -2 * np.pi, 2 * np.pi)
    c[64, C_SEL:C_SEL + 64] = 1.0
    return c.astype(np.float32)


class Buf:
    __slots__ = ("w", "r", "name")

    def __init__(self, name=""):
        self.w = {}
        self.r = {}
        self.name = name


class T:
    def __init__(self, h, name):
        self.h = h
        self.b = Buf(name)

    def __getitem__(self, idx):
        return self.h[idx]


class Eng:
    def __init__(self, name, e, sem):
        self.name, self.e, self.sem, self.cnt, self.seen = name, e, sem, 0, {}
        self.ring = []
        self.di = 0

    def need(self, sem, val):
        if val <= 0 or self.seen.get(id(sem), 0) >= val:
            return
        self.e.wait_ge(sem, val)
        self.seen[id(sem)] = val


class Ctx:
    def __init__(self, nc, es):
        self.nc = nc
        self.es = es
        self.sems = {}
        mk = lambda n: es.enter_context(nc.semaphore(n))
        self.PE = Eng("pe", nc.tensor, mk("s_pe"))
        self.ACT = Eng("act", nc.scalar, mk("s_act"))
        self.DVE = Eng("dve", nc.vector, mk("s_dve"))
        self.POOL = Eng("pool", nc.gpsimd, mk("s_pool"))
        self.SP = Eng("sp", nc.sync, None)
        for i in range(8):
            self.SP.ring.append([mk(f"r_sp{i}"), 0])
            self.POOL.ring.append([mk(f"r_pl{i}"), 0])
        self.engs = [self.PE, self.ACT, self.DVE, self.POOL]
        self.nins = 0

    def _b(self, x):
        return x.b if isinstance(x, T) else x

    def _haz(self, E, reads, writes, own):
        for b in reads:
            for k, (sem, v) in b.w.items():
                E.need(sem, v)
        for b in writes:
            for k, (sem, v) in b.r.items():
                if sem is not own:
                    E.need(sem, v)
            for k, (sem, v) in b.w.items():
                if sem is not own:
                    E.need(sem, v)

    def _rec(self, sem, val, reads, writes):
        for b in reads:
            b.r[id(sem)] = (sem, val)
        for b in writes:
            if b.r:
                b.r = {}
                b.w = {}
            b.w[id(sem)] = (sem, val)

    def op(self, E, fn, reads=(), writes=()):
        reads = [self._b(x) for x in reads]
        writes = [self._b(x) for x in writes]
        self._haz(E, reads, writes, E.sem)
        ins = fn()
        E.cnt += 1
        ins.then_inc(E.sem, 1)
        self._rec(E.sem, E.cnt, reads, writes)
        self.nins += 1

    def dma(self, Q, out, in_, reads=(), writes=(), **kw):
        reads = [self._b(x) for x in reads]
        writes = [self._b(x) for x in writes]
        slot = Q.ring[Q.di % len(Q.ring)]
        Q.di += 1
        Q.need(slot[0], slot[1])
        self._haz(Q, reads, writes, None)
        ins = Q.e.dma_start(out=out, in_=in_, **kw)
        slot[1] += 16
        ins.then_inc(slot[0], 16)
        self._rec(slot[0], slot[1], reads, writes)
        self.nins += 1

    def barrier(self):
        tg = [(E.sem, E.cnt) for E in self.engs]
        for Q in (self.SP, self.POOL):
            tg += [(s[0], s[1]) for s in Q.ring]
        for E in self.engs + [self.SP]:
            for sem, v in tg:
                E.need(sem, v)


def build(nlayers=DEPTH, debug=False, stop=None):
    nc = bass.Bass("TRN2", target_bir_lowering=False)
    dt = nc.dram_tensor
    inp = lambda n, s, d=F32: dt(n, s, d, kind="ExternalInput").ap()
    skind = "ExternalOutput" if debug else "Internal"
    scr = lambda n, s, d=BF16: dt(n, s, d, kind=skind).ap()
    x_in = inp("x", [S, D])
    cT_in = inp("cT", [128, 8])
    pos_in = inp("pos", [1, S], I32)
    consts_in = inp("consts", [128, C_END])
    w_in_all = inp("w_in_all", [DEPTH, D, WIN])
    gn_g = inp("ret_gn_g", [DEPTH, 256])
    qn_gT = inp("qn_gT", [DEPTH, 128, 3])
    kvn_gT = inp("kvn_gT", [DEPTH, 128, 2])
    w_uq_all = inp("w_uq_all", [DEPTH, 384, 768])
    w_ukv_r = inp("w_ukv_r", [DEPTH, 256, 768])
    w_out = inp("w_out", [DEPTH, D, D])
    w_ada = inp("w_ada", [DEPTH, D, 6 * D])
    b_ada = inp("b_ada", [DEPTH, 6 * D])
    ln_in = {k: inp(k, [DEPTH, D]) for k in ("ln1_g", "ln1_b", "ln2_g", "ln2_b")}
    w1d = inp("w1_dense", [2, D, 2816])
    w3d = inp("w3_dense", [2, D, 2816])
    w2d = inp("w2_dense", [2, 2816, D])
    w_router = inp("w_router", [2, D, 8])
    w1m = inp("w1_moe", [2, 8, D, 3584])
    w3m = inp("w3_moe", [2, 8, D, 3584])
    w2m = inp("w2_moe", [2, 8, 3584, D])
    out = dt("out", [S, D], F32, kind="ExternalOutput").ap()
    XS = scr("XS", [S, D], F32)
    X1 = scr("X1", [S, D], F32)
    RQ = scr("RQ", [256, S])
    RQX = scr("RQX", [256, S])
    RK = scr("RK", [256, S])
    RV = scr("RV", [S, 256])
    RG = scr("RG", [S, 256])
    DQ = scr("DQ", [384, S])
    DK = scr("DK", [384, S])
    DV = scr("DV", [S, 384])
    MQ = scr("MQ", [6, 96, S])
    MKN = scr("MKN", [384, S])
    KR = scr("KR", [32, S])
    MV = scr("MV", [S, 384])
    CT = scr("CT", [16, 64, S])
    TB = scr("TB", [4, 128, S], F32)
    XSb, X1b = Buf("XS"), Buf("X1")
    SCR = Buf("scr_mix")
    CTb = Buf("CT")
    TBb = Buf("TB")

    with contextlib.ExitStack() as es:
        K = Ctx(nc, es)
        PE, ACT, DVE, POOL, SP = K.PE, K.ACT, K.DVE, K.POOL, K.SP

        uid = [0]

        def sb(es_, name, shape, dtype):
            uid[0] += 1
            name = f"{name}_{uid[0]}"
            return T(es_.enter_context(nc.sbuf_tensor(name, shape, dtype)), name)

        def ps(es_, name, shape, dtype=F32):
            uid[0] += 1
            name = f"{name}_{uid[0]}"
            return T(es_.enter_context(nc.psum_tensor(name, shape, dtype)), name)

        def act(o, i, func, reads, writes, **kw):
            K.op(ACT, lambda: nc.scalar.activation(out=o, in_=i, func=func, **kw), reads, writes)

        def tt(E, o, a, b, op, reads, writes):
            K.op(E, lambda: E.e.tensor_tensor(out=o, in0=a, in1=b, op=op), reads, writes)

        def ts(E, o, a, s1, s2, op0, op1, reads, writes):
            if s2 is None:
                K.op(E, lambda: E.e.tensor_scalar(out=o, in0=a, scalar1=s1, scalar2=None, op0=op0), reads, writes)
            else:
                K.op(E, lambda: E.e.tensor_scalar(out=o, in0=a, scalar1=s1, scalar2=s2, op0=op0, op1=op1), reads, writes)

        def stt(o, a, sc, b, op0, op1, reads, writes):
            K.op(DVE, lambda: nc.vector.scalar_tensor_tensor(out=o, in0=a, scalar=sc, in1=b, op0=op0, op1=op1), reads, writes)

        def cp(E, o, i, reads, writes):
            K.op(E, lambda: E.e.tensor_copy(out=o, in_=i), reads, writes)

        def mm(o, lhsT, rhs, start, stop, reads, writes):
            K.op(PE, lambda: nc.tensor.matmul(o, lhsT=lhsT, rhs=rhs, start=start, stop=stop), reads, writes)

        def tr(o, i, ident, reads, writes):
            K.op(PE, lambda: nc.tensor.transpose(o, i, ident), reads, writes)

        def recip(o, i, reads, writes):
            K.op(DVE, lambda: nc.vector.reciprocal(out=o, in_=i), reads, writes)

        def rsum(o, i, reads, writes):
            K.op(DVE, lambda: nc.vector.tensor_reduce(out=o, in_=i, axis=AX.X, op=ALU.add), reads, writes)

        cst = sb(es, "cst", [128, C_END], F32)
        K.dma(SP, cst[:], consts_in, writes=[cst])
        ident_bf = sb(es, "ident_bf", [128, 128], BF16)
        band_bf = sb(es, "band_bf", [128, 256], BF16)
        ones_f = sb(es, "ones_f", [128, 128], F32)
        ones_bf = sb(es, "ones_bf", [128, 128], BF16)
        eps_ln = sb(es, "eps_ln", [128, 1], F32)
        eps_rms = sb(es, "eps_rms", [128, 1], F32)
        cp(DVE, ident_bf[:], cst[:, C_ID:C_ID + 128], [cst], [ident_bf])
        cp(DVE, band_bf[:], cst[:, C_LE:C_LE + 256], [cst], [band_bf])
        negband = sb(es, "negband", [128, 256], BF16)
        ts(DVE, negband[:], cst[:, C_LE:C_LE + 256], -1.0, 30000.0, ALU.add, ALU.mult, [cst], [negband])
        K.op(DVE, lambda: nc.vector.memset(ones_f[:], 1.0), [], [ones_f])
        K.op(DVE, lambda: nc.vector.memset(ones_bf[:], 1.0), [], [ones_bf])
        K.op(DVE, lambda: nc.vector.memset(eps_ln[:], LN_EPS), [], [eps_ln])
        K.op(DVE, lambda: nc.vector.memset(eps_rms[:], RMS_EPS), [], [eps_rms])
        ident_f = cst
        silu_cT = sb(es, "silu_cT", [128, 8], F32)
        K.dma(SP, silu_cT[:], cT_in, writes=[silu_cT])
        act(silu_cT[:], silu_cT[:], AF.Silu, [silu_cT], [silu_cT])
        modcol = sb(es, "modcol", [128, 32], F32)
        g1p_bc = sb(es, "g1p_bc", [128, D], F32)
        g2p_bc = sb(es, "g2p_bc", [128, D], F32)
        lnp = {k: sb(es, "bc_" + k, [128, D], F32) for k in ln_in}

        with contextlib.ExitStack() as e1:
            posi = sb(e1, "posi", [128, S], I32)
            yv = sb(e1, "yv", [128, S], F32)
            ki = sb(e1, "ki", [128, S], I32)
            fr = sb(e1, "fr", [128, S], F32)
            m_ = sb(e1, "m_", [128, S], F32)
            K.dma(SP, posi[:], pos_in.broadcast_to([128, S]), writes=[posi])
            for ti, (cif, csg, shift) in enumerate(((C_IF64, None, 0.25), (C_IF64, C_SG64, 0.0), (C_IF32, None, 0.25), (C_IF32, C_SG32, 0.0))):
                cp(DVE, fr[:], posi[:], [posi], [fr])
                ts(DVE, yv[:], fr[:], cst[:, cif:cif + 1], shift, ALU.mult, ALU.add, [fr, cst], [yv])
                cp(DVE, ki[:], yv[:], [yv], [ki])
                cp(DVE, fr[:], ki[:], [ki], [fr])
                tt(DVE, fr[:], yv[:], fr[:], ALU.subtract, [yv, fr], [fr])
                ts(DVE, m_[:], fr[:], 0.5, None, ALU.is_gt, None, [fr], [m_])
                tt(DVE, fr[:], fr[:], m_[:], ALU.subtract, [fr, m_], [fr])
                ts(DVE, m_[:], fr[:], -0.5, None, ALU.is_lt, None, [fr], [m_])
                tt(DVE, fr[:], fr[:], m_[:], ALU.add, [fr, m_], [fr])
                if csg is None:
                    act(yv[:], fr[:], AF.Sin, [fr], [yv], scale=float(2 * np.pi))
                else:
                    act(yv[:], fr[:], AF.Sin, [fr, cst], [yv], scale=cst[:, csg:csg + 1])
                K.dma(SP, TB[ti], yv[:], reads=[yv], writes=[TBb])
            K.barrier()

        def load_hT(es_, xsrc, xbuf, blk, hT, col_sc, col_sh, xt, pst, hT32=None):
            for j in range(4):
                t = blk * 4 + j
                xx = xt[j % 2]
                K.dma(SP, xx[:], xsrc[t * 128:(t + 1) * 128, :], reads=[xbuf], writes=[xx])
                for k in range(8):
                    tr(pst[k][:, j * 128:(j + 1) * 128], xx[:, k * 128:(k + 1) * 128], cst[:, C_ID:C_ID + 128], [xx, cst], [pst[k]])
            for k in range(8):
                act(hT[:, k, :], pst[k][:], AF.Identity, [pst[k], modcol], [hT],
                    scale=modcol[:, col_sc + k:col_sc + k + 1], bias=modcol[:, col_sh + k:col_sh + k + 1])
                if hT32 is not None:
                    act(hT32[:, k, :], pst[k][:], AF.Identity, [pst[k], modcol], [hT32],
                        scale=modcol[:, col_sc + k:col_sc + k + 1], bias=modcol[:, col_sh + k:col_sh + k + 1])

        def layer_norm_store(u, gk, bk, dst, dstbuf, t, tmp, st):
            rsum(st[:, 0:1], u[:], [u], [st])
            act(tmp[:], u[:], AF.Square, [u], [tmp])
            rsum(st[:, 1:2], tmp[:], [tmp], [st])
            ts(DVE, st[:, 2:3], st[:, 0:1], 1.0 / D, None, ALU.mult, None, [st], [st])
            tt(DVE, st[:, 3:4], st[:, 2:3], st[:, 2:3], ALU.mult, [st], [st])
            stt(st[:, 4:5], st[:, 1:2], 1.0 / D, st[:, 3:4], ALU.mult, ALU.subtract, [st], [st])
            act(st[:, 5:6], st[:, 4:5], AF.Sqrt, [st, eps_ln], [st], bias=eps_ln[:, 0:1], scale=1.0)
            recip(st[:, 6:7], st[:, 5:6], [st], [st])
            stt(st[:, 7:8], st[:, 2:3], -1.0, st[:, 6:7], ALU.mult, ALU.mult, [st], [st])
            act(tmp[:], u[:], AF.Identity, [u, st], [tmp], scale=st[:, 6:7], bias=st[:, 7:8])
            tt(POOL, tmp[:], tmp[:], lnp[gk][:], ALU.mult, [tmp, lnp[gk]], [tmp])
            tt(POOL, u[:], tmp[:], lnp[bk][:], ALU.add, [tmp, lnp[bk]], [u])
            K.dma(SP, dst[t * 128:(t + 1) * 128, :], u[:], reads=[u], writes=[dstbuf])

        for l in range(nlayers):
            xsrc = x_in if l == 0 else XS
            xdst = out if l == nlayers - 1 else XS
            with contextlib.ExitStack() as e1:
                wad = [sb(e1, f"wad{i}", [128, 8, 512], F32) for i in range(2)]
                modrow = sb(e1, "modrow", [1, 6 * D], F32)
                brow = sb(e1, "brow", [1, 6 * D], F32)
                pm_ = [ps(e1, f"pm{i}", [128, 512]) for i in range(2)]
                pcol = ps(e1, "pcol", [128, 32])
                K.dma(SP, brow[:], b_ada[l:l + 1, :], writes=[brow])
                for k in lnp:
                    K.dma(SP, lnp[k][:], ln_in[k][l:l + 1, :].broadcast_to([128, D]), writes=[lnp[k]])
                for cb in range(12):
                    wv = wad[cb % 2]
                    K.dma(SP, wv[:], w_ada[l, :, cb * 512:(cb + 1) * 512].rearrange("(k p) n -> p k n", p=128), writes=[wv])
                    pp = pm_[cb % 2]
                    for k in range(8):
                        mm(pp[0:1, :], silu_cT[:, k:k + 1], wv[:, k, :], k == 0, k == 7, [silu_cT, wv], [pp])
                    tt(DVE, modrow[0:1, cb * 512:(cb + 1) * 512], pp[0:1, :], brow[0:1, cb * 512:(cb + 1) * 512], ALU.add,
                       [pp, brow], [modrow])
                for gi, (base, dst) in enumerate(((2 * D, g1p_bc), (5 * D, g2p_bc))):
                    for hf in range(2):
                        pp = pm_[hf]
                        mm(pp[:, :], ones_f[0:1, :], modrow[0:1, base + hf * 512: base + (hf + 1) * 512], True, True, [ones_f, modrow], [pp])
                        ts(DVE, dst[:, hf * 512:(hf + 1) * 512], pp[:], 1.0, None, ALU.add, None, [pp], [dst])
                for ci, base in enumerate((0, D, 3 * D, 4 * D)):
                    for k in range(8):
                        mm(pcol[:, ci * 8 + k: ci * 8 + k + 1], modrow[0:1, base + k * 128: base + (k + 1) * 128], ones_f[0:1, 0:1],
                           True, True, [modrow, ones_f], [pcol])
                cp(DVE, modcol[:], pcol[:], [pcol], [modcol])
                ts(DVE, modcol[:, 8:16], modcol[:, 8:16], 1.0, None, ALU.add, None, [modcol], [modcol])
                ts(DVE, modcol[:, 24:32], modcol[:, 24:32], 1.0, None, ALU.add, None, [modcol], [modcol])
                K.barrier()
            if stop == "M":
                break

            with contextlib.ExitStack() as e1:
                win = sb(e1, "win", [128, 8, WIN], BF16)
                for k in range(8):
                    K.dma(POOL, win[:, k, :], w_in_all[l, k * 128:(k + 1) * 128, :], writes=[win], max_dma_last_dim=4096)
                wuq = sb(e1, "wuq", [128, 3, 768], BF16)
                wukv = sb(e1, "wukv", [128, 2, 768], BF16)
                wq32 = sb(e1, "wq32", [128, 3, 768], F32)
                wkv32 = sb(e1, "wkv32", [128, 2, 768], F32)
                qng = sb(e1, "qng", [128, 3], F32)
                kvg = sb(e1, "kvg", [128, 2], F32)
                K.dma(SP, wq32[:], w_uq_all[l].rearrange("(k p) n -> p k n", p=128), writes=[wq32])
                K.dma(SP, wkv32[:], w_ukv_r[l].rearrange("(k p) n -> p k n", p=128), writes=[wkv32])
                K.dma(SP, qng[:], qn_gT[l], writes=[qng])
                K.dma(SP, kvg[:], kvn_gT[l], writes=[kvg])
                for k in range(3):
                    ts(DVE, wuq[:, k, :], wq32[:, k, :], qng[:, k:k + 1], None, ALU.mult, None, [wq32, qng], [wuq])
                for k in range(2):
                    ts(DVE, wukv[:, k, :], wkv32[:, k, :], kvg[:, k:k + 1], None, ALU.mult, None, [wkv32, kvg], [wukv])
                hT = sb(e1, "hT", [128, 8, 512], BF16)
                xt = [sb(e1, f"xt{i}", [128, D], F32) for i in range(2)]
                tb = sb(e1, "tb", [128, 4, 512], F32)
                fm = sb(e1, "fm", [128, 12, 512], BF16)
                tmo = sb(e1, "tmo", [128, 4, 896], BF16)
                t1 = sb(e1, "t1", [128, 512], F32)
                t2 = sb(e1, "t2", [128, 512], F32)
                cqT = sb(e1, "cqT", [128, 5, 512], BF16)
                sq = sb(e1, "sq", [128, 5, 512], F32)
                rs_q = sb(e1, "rs_q", [128, 512], F32)
                rs_kv = sb(e1, "rs_kv", [128, 512], F32)
                rs_col = sb(e1, "rs_col", [128, 4], F32)
                mqs = sb(e1, "mqs", [96, 6, 512], BF16)
                mkn = sb(e1, "mkn", [128, 3, 512], BF16)
                krs = sb(e1, "krs", [96, 512], BF16)
                mvs = sb(e1, "mvs", [128, 4, 384], BF16)
                with contextlib.ExitStack() as e2:
                    pst = [ps(e2, f"pst{i}", [128, 512]) for i in range(8)]
                    for blk in range(NB):
                        c0, c1 = blk * 512, (blk + 1) * 512
                        K.dma(SP, tb[:], TB[:, :, c0:c1].rearrange("a p t -> p a t"), reads=[TBb], writes=[tb])
                        load_hT(e2, xsrc, XSb, blk, hT, 8, 0, xt, pst)
                        bank = [0]

                        def nb():
                            bank[0] = (bank[0] + 1) % 8
                            return pst[bank[0]]
                        fm_off = [0, 128, 256, 384, 512, 640, 768, 896, 1024, 1152]
                        for ci in range(10):
                            pa, pb = nb(), nb()
                            fo = fm_off[ci]
                            for k in range(8):
                                mm(pa[:], win[:, k, fo:fo + 128], hT[:, k, :], k == 0, k == 7, [win, hT], [pa])
                            for k in range(8):
                                mm(pb[:], win[:, k, NFM + fo:NFM + fo + 128], hT[:, k, :], k == 0, k == 7, [win, hT], [pb])
                            tt(DVE, t1[:], pa[:], tb[:, 0, :], ALU.mult, [pa, tb], [t1])
                            tt(DVE, t2[:], pb[:], tb[:, 1, :], ALU.mult, [pb, tb], [t2])
                            slot = ci if ci < 2 else ci + 2
                            tt(DVE, fm[:, slot, :], t1[:], t2[:], ALU.add, [t1, t2], [fm])
                            if ci < 2:
                                for hh in range(2):
                                    h = ci * 2 + hh
                                    tt(POOL, fm[hh * 64:(hh + 1) * 64, 2 + ci, :].rearrange("p (c i) -> p c i", i=128),
                                       fm[hh * 64:(hh + 1) * 64, slot, :].rearrange("p (c i) -> p c i", i=128),
                                       cst[hh * 64:(hh + 1) * 64, C_XI + 128 * h:C_XI + 128 * (h + 1)].unsqueeze(1).broadcast_to([64, 4, 128]),
                                       ALU.mult, [fm, cst], [fm])
                        for ci in range(5):
                            pa = nb()
                            fo = 1280 + ci * 128
                            for k in range(8):
                                mm(pa[:], win[:, k, fo:fo + 128], hT[:, k, :], k == 0, k == 7, [win, hT], [pa])
                            act(cqT[:, ci, :], pa[:], AF.Copy, [pa], [cqT])
                            act(sq[:, ci, :], pa[:], AF.Square, [pa], [sq])
                        for (lo, n, rs, dim) in ((0, 3, rs_q, 384.0), (3, 2, rs_kv, 256.0)):
                            pa = nb()
                            for k in range(n):
                                mm(pa[:], ones_f[:], sq[:, lo + k, :], k == 0, k == n - 1, [ones_f, sq], [pa])
                            act(rs[:], pa[:], AF.Sqrt, [pa, eps_rms], [rs], scale=1.0 / dim, bias=eps_rms[:, 0:1])
                            recip(rs[:], rs[:], [rs], [rs])
                        pa, pb = nb(), nb()
                        for k in range(8):
                            mm(pa[64:96, :], win[:, k, 1920:1952], hT[:, k, :], k == 0, k == 7, [win, hT], [pa])
                        for k in range(8):
                            mm(pb[64:96, :], win[:, k, NFM + 1280:NFM + 1312], hT[:, k, :], k == 0, k == 7, [win, hT], [pb])
                        tt(DVE, t1[64:96, :], pa[64:96, :], tb[64:96, 2, :], ALU.mult, [pa, tb], [t1])
                        tt(DVE, t2[64:96, :], pb[64:96, :], tb[64:96, 3, :], ALU.mult, [pb, tb], [t2])
                        tt(DVE, krs[64:96, :], t1[64:96, :], t2[64:96, :], ALU.add, [t1, t2], [krs])
                        for h in range(6):
                            pa, pb = nb(), nb()
                            for k in range(3):
                                mm(pa[0:96, :], wuq[:, k, 96 * h:96 * h + 96], cqT[:, k, :], k == 0, k == 2, [wuq, cqT], [pa])
                            for k in range(3):
                                mm(pb[64:96, :], wuq[:, k, 576 + 32 * h:576 + 32 * h + 32], cqT[:, k, :], k == 0, k == 2, [wuq, cqT], [pb])
                            tt(DVE, mqs[0:64, h, :], pa[0:64, :], rs_q[0:64, :], ALU.mult, [pa, rs_q], [mqs])
                            tt(DVE, t1[64:96, :], pa[64:96, :], tb[64:96, 2, :], ALU.mult, [pa, tb], [t1])
                            tt(DVE, t2[64:96, :], pb[64:96, :], tb[64:96, 3, :], ALU.mult, [pb, tb], [t2])
                            tt(DVE, t1[64:96, :], t1[64:96, :], t2[64:96, :], ALU.add, [t1, t2], [t1])
                            tt(DVE, mqs[64:96, h, :], t1[64:96, :], rs_q[64:96, :], ALU.mult, [t1, rs_q], [mqs])
                        for c in range(3):
                            pa = nb()
                            for k in range(2):
                                mm(pa[:], wukv[:, k, c * 128:(c + 1) * 128], cqT[:, 3 + k, :], k == 0, k == 1, [wukv, cqT], [pa])
                            tt(DVE, mkn[:, c, :], pa[:], rs_kv[:], ALU.mult, [pa, rs_kv], [mkn])
                        for j in range(4):
                            tsl = slice(j * 128, (j + 1) * 128)
                            pa, pb, pc, pd = nb(), nb(), nb(), nb()
                            for k in range(8):
                                mm(pa[:], hT[:, k, tsl], win[:, k, NFM + NPM:NFM + NPM + 512], k == 0, k == 7, [win, hT], [pa])
                            for k in range(8):
                                mm(pb[:, 0:384], hT[:, k, tsl], win[:, k, NFM + NPM + 512:NFM + NPM + 896], k == 0, k == 7, [win, hT], [pb])
                            for k in range(2):
                                mm(pc[:, 0:384], cqT[:, 3 + k, tsl], wukv[:, k, 384:768], k == 0, k == 1, [wukv, cqT], [pc])
                            for k in range(2):
                                mm(pd[:, 0:1], sq[:, 3 + k, tsl], ones_f[:, 0:1], k == 0, k == 1, [sq, ones_f], [pd])
                            act(tmo[:, j, 0:256], pa[:, 0:256], AF.Copy, [pa], [tmo])
                            act(tmo[:, j, 256:512], pa[:, 256:512], AF.Silu, [pa], [tmo])
                            act(tmo[:, j, 512:896], pb[:, 0:384], AF.Copy, [pb], [tmo])
                            act(rs_col[:, j:j + 1], pd[:, 0:1], AF.Sqrt, [pd, eps_rms], [rs_col], scale=1.0 / 256.0, bias=eps_rms[:, 0:1])
                            recip(rs_col[:, j:j + 1], rs_col[:, j:j + 1], [rs_col], [rs_col])
                            ts(DVE, mvs[:, j, :], pc[:, 0:384], rs_col[:, j:j + 1], None, ALU.mult, None, [pc, rs_col], [mvs])
                        def fmv(dst, n):
                            return dst.rearrange("(c p) t -> p c t", p=128)[:, :, c0:c1]
                        K.dma(SP, fmv(RQ, 2), fm[:, 0:2, :], reads=[fm], writes=[SCR])
                        K.dma(SP, fmv(RQX, 2), fm[:, 2:4, :], reads=[fm], writes=[SCR])
                        K.dma(SP, fmv(RK, 2), fm[:, 4:6, :], reads=[fm], writes=[SCR])
                        K.dma(SP, fmv(DQ, 3), fm[:, 6:9, :], reads=[fm], writes=[SCR])
                        K.dma(SP, fmv(DK, 3), fm[:, 9:12, :], reads=[fm], writes=[SCR])
                        K.dma(SP, MQ[:, :, c0:c1].rearrange("h d t -> d h t"), mqs[:], reads=[mqs], writes=[SCR])
                        K.dma(SP, fmv(MKN, 3), mkn[:], reads=[mkn], writes=[SCR])
                        K.dma(SP, KR[:, c0:c1], krs[64:96, :], reads=[krs], writes=[SCR])
                        tmv = lambda dst: dst[c0:c1, :].rearrange("(j p) e -> p j e", p=128)
                        K.dma(SP, tmv(RV), tmo[:, :, 0:256], reads=[tmo], writes=[SCR])
                        K.dma(SP, tmv(RG), tmo[:, :, 256:512], reads=[tmo], writes=[SCR])
                        K.dma(SP, tmv(DV), tmo[:, :, 512:896], reads=[tmo], writes=[SCR])
                        K.dma(SP, tmv(MV), mvs[:], reads=[mvs], writes=[SCR])
                K.barrier()
            if stop == "A":
                break

            with contextlib.ExitStack() as e1:
                r_sets = [dict(qT=sb(e1, f"r_qT{i}", [64, S], BF16), qxT=sb(e1, f"r_qxT{i}", [64, S], BF16),
                               kT=sb(e1, f"r_kT{i}", [64, S], BF16), vv=sb(e1, f"r_v{i}", [128, NT, 64], BF16),
                               gg=sb(e1, f"r_g{i}", [128, NT, 64], BF16)) for i in range(2)]

                def r_load(h):
                    st_ = r_sets[h % 2]
                    hs_ = slice(h * 64, (h + 1) * 64)
                    K.dma(SP, st_["qT"][:], RQ[hs_, :], reads=[SCR], writes=[st_["qT"]])
                    K.dma(SP, st_["qxT"][:], RQX[hs_, :], reads=[SCR], writes=[st_["qxT"]])
                    K.dma(SP, st_["kT"][:], RK[hs_, :], reads=[SCR], writes=[st_["kT"]])
                    K.dma(SP, st_["vv"][:], RV[:, hs_].rearrange("(t p) e -> p t e", p=128), reads=[SCR], writes=[st_["vv"]])
                    K.dma(SP, st_["gg"][:], RG[:, hs_].rearrange("(t p) e -> p t e", p=128), reads=[SCR], writes=[st_["gg"]])
                kz = sb(e1, "r_kz", [128, NT, 64], BF16)
                prev = sb(e1, "r_prev", [64, NT, 64], BF16)
                state = sb(e1, "r_state", [64, 64], F32)
                sTm = [sb(e1, f"r_sTm{i}", [128, 4, 128], BF16) for i in range(2)]
                osb = sb(e1, "r_osb", [128, 8, 64], F32)
                osq = sb(e1, "r_osq", [128, 8, 64], F32)
                obf = sb(e1, "r_obf", [128, 8, 64], BF16)
                st = sb(e1, "r_st", [128, 6, 8], F32)
                gng = sb(e1, "r_gng", [128, 256], F32)
                ost = sb(e1, "r_ost", [64, 1024], BF16)
                ptr = ps(e1, "r_ptr", [128, 1024], BF16)
                pkv = [ps(e1, f"r_pkv{i}", [64, 512]) for i in range(1)]
                pss = [ps(e1, f"r_pss{i}", [128, 512]) for i in range(2)]
                pso = ps(e1, "r_pso", [128, 512])
                pot = ps(e1, "r_pot", [64, 1024], BF16)
                K.dma(SP, gng[:], gn_g[l:l + 1, :].broadcast_to([128, 256]), writes=[gng])
                r_load(0)
                for h in range(4):
                    lg = float(np.log(1.0 - 2.0 ** (-5.0 - h)))
                    gC = float(np.exp(128.0 * lg))
                    hs = slice(h * 64, (h + 1) * 64)
                    if h + 1 < 4:
                        r_load(h + 1)
                    qT, qxT, kT, vv, gg = (r_sets[h % 2][k_] for k_ in ("qT", "qxT", "kT", "vv", "gg"))
                    for g8 in range(4):
                        for c in range(8):
                            n = g8 * 8 + c
                            tr(ptr[:, c * 64:(c + 1) * 64], kT[:, n * 128:(n + 1) * 128], ident_bf[0:64, 0:64], [kT, ident_bf], [ptr])
                        act(kz[:, g8 * 8:(g8 + 1) * 8, :], ptr[:, 0:512].rearrange("p (c e) -> p c e", e=64), AF.Identity, [ptr, cst], [kz],
                            scale=cst[:, C_ZE + h:C_ZE + h + 1])
                    K.op(DVE, lambda: nc.vector.memset(state[:], 0.0), [], [state])
                    for g8 in range(4):
                        pk = pkv[0]
                        for c in range(8):
                            n = g8 * 8 + c
                            mm(pk[:, c * 64:(c + 1) * 64], kz[:, n, :], vv[:, n, :], True, True, [kz, vv], [pk])
                        for c in range(8):
                            n = g8 * 8 + c
                            cp(DVE, prev[:, n, :], state[:], [state], [prev])
                            stt(state[:], state[:], gC, pk[:, c * 64:(c + 1) * 64], ALU.mult, ALU.add, [state, pk], [state])
                    for g8 in range(4):
                        for half in range(2):
                            pS = pss[half]
                            for c in range(4):
                                n = g8 * 8 + half * 4 + c
                                cs = slice(n * 128, (n + 1) * 128)
                                mm(pS[:, c * 128:(c + 1) * 128], kT[:, cs], qT[:, cs], True, True, [kT, qT], [pS])
                            tt(DVE, sTm[half][:], pS[:].rearrange("p (c i) -> p c i", i=128),
                               cst[:, C_DM + 128 * h:C_DM + 128 * (h + 1)].unsqueeze(1).broadcast_to([128, 4, 128]),
                               ALU.mult, [pS, cst], [sTm[half]])
                            for c in range(4):
                                n = g8 * 8 + half * 4 + c
                                cc = half * 4 + c
                                cs = slice(n * 128, (n + 1) * 128)
                                mm(pso[:, cc * 64:(cc + 1) * 64], sTm[half][:, c, :], vv[:, n, :], True, False, [sTm[half], vv], [pso])
                                mm(pso[:, cc * 64:(cc + 1) * 64], qxT[:, cs], prev[:, n, :], False, True, [qxT, prev], [pso])
                        p3 = pso[:].rearrange("p (c e) -> p c e", e=64)
                        act(osb[:], p3, AF.Copy, [pso], [osb])
                        act(osq[:], p3, AF.Square, [pso], [osq])
                        rsum(st[:, 0, :], osb[:], [osb], [st])
                        rsum(st[:, 1, :], osq[:], [osq], [st])
                        ts(DVE, st[:, 2, :], st[:, 0, :], 1.0 / 64, None, ALU.mult, None, [st], [st])
                        tt(DVE, st[:, 3, :], st[:, 2, :], st[:, 2, :], ALU.mult, [st], [st])
                        stt(st[:, 4, :], st[:, 1, :], 1.0 / 64, st[:, 3, :], ALU.mult, ALU.subtract, [st], [st])
                        act(st[:, 5, :], st[:, 4, :], AF.Sqrt, [st, eps_ln], [st], bias=eps_ln[:, 0:1], scale=1.0)
                        recip(st[:, 5, :], st[:, 5, :], [st], [st])
                        tt(DVE, osb[:], osb[:], st[:, 2, :].unsqueeze(2).broadcast_to([128, 8, 64]), ALU.subtract, [osb, st], [osb])
                        tt(DVE, osb[:], osb[:], st[:, 5, :].unsqueeze(2).broadcast_to([128, 8, 64]), ALU.mult, [osb, st], [osb])
                        tt(DVE, osb[:], osb[:], gng[:, hs].unsqueeze(1).broadcast_to([128, 8, 64]), ALU.mult, [osb, gng], [osb])
                        tt(DVE, obf[:], osb[:], gg[:, g8 * 8:(g8 + 1) * 8, :], ALU.mult, [osb, gg], [obf])
                        for c in range(8):
                            tr(pot[:, c * 128:(c + 1) * 128], obf[:, c, :], ident_bf[:], [obf, ident_bf], [pot])
                        act(ost[:], pot[:], AF.Copy, [pot], [ost])
                        K.dma(SP, CT[h, :, g8 * 1024:(g8 + 1) * 1024], ost[:], reads=[ost], writes=[CTb])
                K.barrier()
            if stop == "R":
                break

            def normalize_store(e_, oacc, slot, nrm_ps, rcp, ostg):
                for qb in range(NB):
                    cs = slice(qb * 512, (qb + 1) * 512)
                    pn = nrm_ps[qb % 2]
                    mm(pn[0:64, :], cst[0:65, C_SEL:C_SEL + 64], oacc[0:65, cs], True, True, [cst, oacc], [pn])
                    recip(rcp[:, :], pn[0:64, :], [pn], [rcp])
                    tt(DVE, ostg[:, cs], oacc[0:64, cs], rcp[:, :], ALU.mult, [oacc, rcp], [ostg])
                K.dma(SP, CT[slot], ostg[:], reads=[ostg], writes=[CTb])

            with contextlib.ExitStack() as e1:
                d_sets = [dict(qT=sb(e1, f"d_qT{i}", [64, S], BF16), kT=sb(e1, f"d_kT{i}", [64, S], BF16),
                               vr=[sb(e1, f"d_v{i}_{j}", [128, NT, 65], BF16) for j in range(3)]) for i in range(2)]

                def d_load(h):
                    st_ = d_sets[h % 2]
                    hs_ = slice(h * 64, (h + 1) * 64)
                    K.dma(SP, st_["qT"][:], DQ[hs_, :], reads=[SCR], writes=[st_["qT"]])
                    K.dma(SP, st_["kT"][:], DK[hs_, :], reads=[SCR], writes=[st_["kT"]])
                    for pi_, r_ in enumerate((1, 4, 16)):
                        nkt_ = NT // r_
                        for rho_ in range(r_):
                            src = DV[:, hs_].rearrange("(kt j rr) e -> rr j kt e", j=128, rr=r_)[rho_]
                            K.dma(SP, st_["vr"][pi_][:, rho_ * nkt_:(rho_ + 1) * nkt_, 0:64], src, reads=[SCR], writes=[st_["vr"][pi_]])
                oacc = sb(e1, "d_oacc", [65, S], F32)
                rcp = sb(e1, "d_rcp", [64, 512], F32)
                ostg = sb(e1, "d_ostg", [64, S], BF16)
                pT = [sb(e1, f"d_pT{i}", [128, 256], BF16) for i in range(3)]
                pss = [ps(e1, f"d_pss{i}", [128, 512]) for i in range(3)]
                pso = [ps(e1, f"d_pso{i}", [128, 512]) for i in range(2)]
                nrm = [ps(e1, f"d_nrm{i}", [128, 512]) for i in range(2)]
                for st_ in d_sets:
                    for i in range(3):
                        K.op(POOL, lambda i=i, st_=st_: nc.gpsimd.memset(st_["vr"][i][:], 1.0), [], [st_["vr"][i]])
                it = 0
                d_load(0)
                for h in range(6):
                    hs = slice(h * 64, (h + 1) * 64)
                    if h + 1 < 6:
                        d_load(h + 1)
                    qT, kT, vr = d_sets[h % 2]["qT"], d_sets[h % 2]["kT"], d_sets[h % 2]["vr"]
                    for pi, r in enumerate((1, 4, 16)):
                        nkt = NT // r
                        L = S // r
                        qv = qT[:].rearrange("d (m rr) -> d rr m", rr=r)
                        kv_ = kT[:].rearrange("d (m rr) -> d rr m", rr=r)
                        ov = oacc[:].rearrange("d (m rr) -> d rr m", rr=r)
                        for rho in range(r):
                            for kt in range(nkt):
                                nq = 256 if kt < nkt - 1 else 128
                                pS = pss[it % 3]
                                pt = pT[it % 3]
                                it += 1
                                mm(pS[:, 0:nq], kv_[:, rho, kt * 128:(kt + 1) * 128], qv[:, rho, kt * 128:kt * 128 + nq], True, False, [kT, qT], [pS])
                                mm(pS[:, 0:nq], ident_bf[:], negband[:, 0:nq], False, True, [ident_bf, negband], [pS])
                                act(pt[:, 0:nq], pS[:, 0:nq], AF.Exp, [pS], [pt], scale=0.125)
                                bk = kt // 4
                                po = pso[bk % 2]
                                vt = vr[pi][:, rho * nkt + kt, :]
                                c = kt % 4
                                mm(po[0:65, c * 128:(c + 1) * 128], vt, pt[:, 0:128], kt == 0, True, [vr[pi], pt], [po])
                                if nq == 256:
                                    bk2 = (kt + 1) // 4
                                    po2 = pso[bk2 % 2]
                                    c2 = (kt + 1) % 4
                                    mm(po2[0:65, c2 * 128:(c2 + 1) * 128], vt, pt[:, 128:256], True, False, [vr[pi], pt], [po2])
                                if c == 3 or kt == nkt - 1:
                                    w = (c + 1) * 128
                                    m0 = bk * 512
                                    if pi == 0:
                                        act(ov[0:65, rho, m0:m0 + w], po[0:65, 0:w], AF.Copy, [po], [oacc])
                                    else:
                                        tt(DVE, ov[0:65, rho, m0:m0 + w], po[0:65, 0:w], ov[0:65, rho, m0:m0 + w], ALU.add, [po, oacc], [oacc])
                    normalize_store(e1, oacc, 4 + h, nrm, rcp, ostg)
                K.barrier()
            if stop == "D":
                break

            with contextlib.ExitStack() as e1:
                m_sets = [dict(qT=sb(e1, f"m_qT{i}", [96, S], BF16), kT=sb(e1, f"m_kT{i}", [96, S], BF16),
                               vt=sb(e1, f"m_v{i}", [128, NT, 65], BF16)) for i in range(2)]

                def m_load(h):
                    st_ = m_sets[h % 2]
                    K.dma(SP, st_["qT"][:], MQ[h], reads=[SCR], writes=[st_["qT"]])
                    K.dma(SP, st_["kT"][0:64, :], MKN[h * 64:(h + 1) * 64, :], reads=[SCR], writes=[st_["kT"]])
                    K.dma(SP, st_["kT"][64:96, :], KR, reads=[SCR], writes=[st_["kT"]])
                    K.dma(SP, st_["vt"][:, :, 0:64], MV[:, h * 64:(h + 1) * 64].rearrange("(t p) e -> p t e", p=128), reads=[SCR], writes=[st_["vt"]])
                oacc = sb(e1, "m_oacc", [65, S], F32)
                rcp = sb(e1, "m_rcp", [64, 512], F32)
                ostg = sb(e1, "m_ostg", [64, S], BF16)
                pT = [sb(e1, f"m_pT{i}", [128, 512], BF16) for i in range(3)]
                pss = [ps(e1, f"m_pss{i}", [128, 512]) for i in range(3)]
                pso = [ps(e1, f"m_pso{i}", [128, 512]) for i in range(2)]
                nrm = [ps(e1, f"m_nrm{i}", [128, 512]) for i in range(2)]
                for st_ in m_sets:
                    K.op(POOL, lambda st_=st_: nc.gpsimd.memset(st_["vt"][:], 1.0), [], [st_["vt"]])
                sc = float(96 ** -0.5)
                it = 0
                m_load(0)
                for h in range(6):
                    if h + 1 < 6:
                        m_load(h + 1)
                    qT, kT, vt_ = m_sets[h % 2]["qT"], m_sets[h % 2]["kT"], m_sets[h % 2]["vt"]
                    for qb in range(NB):
                        po = pso[qb % 2]
                        nk = 4 * qb + 4
                        for kt in range(nk):
                            j = kt - 4 * qb
                            q0 = 0 if j < 0 else 128 * j
                            pS = pss[it % 3]
                            pt = pT[it % 3]
                            it += 1
                            mm(pS[:, q0:512], kT[:, kt * 128:(kt + 1) * 128], qT[:, qb * 512 + q0:(qb + 1) * 512], True, j < 0, [kT, qT], [pS])
                            if j >= 0:
                                mm(pS[:, q0:q0 + 128], ident_bf[:], negband[:, 0:128], False, True, [ident_bf, negband], [pS])
                            act(pt[:, q0:512], pS[:, q0:512], AF.Exp, [pS], [pt], scale=sc)
                            mm(po[0:65, q0:512], vt_[:, kt, :], pt[:, q0:512], kt == 0, kt == nk - 1, [vt_, pt], [po])
                        act(oacc[0:65, qb * 512:(qb + 1) * 512], po[0:65, :], AF.Copy, [po], [oacc])
                    normalize_store(e1, oacc, 10 + h, nrm, rcp, ostg)
                K.barrier()
            if stop == "ML":
                break

            with contextlib.ExitStack() as e1:
                wo32 = sb(e1, "wo32", [128, 8, D], F32)
                wo = sb(e1, "wo", [128, 8, D], BF16)
                K.dma(SP, wo32[:], w_out[l].rearrange("(c p) n -> p c n", p=128), writes=[wo32])
                for c in range(8):
                    tt(POOL if c % 2 else DVE, wo[:, c, :], wo32[:, c, :], g1p_bc[:], ALU.mult, [wo32, g1p_bc], [wo])
                ct = [sb(e1, f"o_ct{i}", [128, 8, 512], BF16) for i in range(2)]
                xt = [sb(e1, f"o_xt{i}", [128, D], F32) for i in range(2)]
                u = [sb(e1, f"o_u{i}", [128, D], F32) for i in range(2)]
                tmp = [sb(e1, f"o_tmp{i}", [128, D], F32) for i in range(2)]
                st = [sb(e1, f"o_st{i}", [128, 8], F32) for i in range(2)]
                py = [ps(e1, f"o_py{i}", [128, 512]) for i in range(4)]
                for blk in range(NB):
                    cb = ct[blk % 2]
                    K.dma(SP, cb[:], CT[:, :, blk * 512:(blk + 1) * 512].rearrange("(c s) d t -> (s d) c t", s=2), reads=[CTb], writes=[cb])
                    for j in range(4):
                        t = blk * 4 + j
                        xx = xt[t % 2]
                        uu = u[t % 2]
                        K.dma(SP, xx[:], xsrc[t * 128:(t + 1) * 128, :], reads=[XSb], writes=[xx])
                        for hf in range(2):
                            pp = py[(t % 2) * 2 + hf]
                            for c in range(8):
                                mm(pp[:], cb[:, c, j * 128:(j + 1) * 128], wo[:, c, hf * 512:(hf + 1) * 512], c == 0, c == 7, [cb, wo], [pp])
                            stt(uu[:, hf * 512:(hf + 1) * 512], xx[:, hf * 512:(hf + 1) * 512], ALPHA, pp[:], ALU.mult, ALU.add, [xx, pp], [uu])
                        layer_norm_store(uu, "ln1_g", "ln1_b", X1, X1b, t, tmp[t % 2], st[t % 2])
                K.barrier()
            if stop == "O":
                break

            moe = (l % 2 == 1)
            li = l // 2
            NE = 8 if moe else 1
            DFF = 3584 if moe else 2816
            NG = DFF // 256
            with contextlib.ExitStack() as e1:
                hT = sb(e1, "f_hT", [128, 8, 2048], BF16)
                yacc = sb(e1, "f_yacc", [128, 16, D], F32)
                gT = [sb(e1, f"f_gT{i}", [128, 2, 2048], BF16) for i in range(2)]
                wst = sb(e1, "f_wst", [128, 3, 2048], F32)
                wb = [sb(e1, f"f_wb{i}", [128, 3, 2048], BF16) for i in range(2)]
                xt = [sb(e1, f"f_xt{i}", [128, D], F32) for i in range(2)]
                sa = [sb(e1, f"f_sa{i}", [128, 512], BF16) for i in range(2)]
                comb = sb(e1, "f_comb", [128, 16, 8], F32)
                lg = sb(e1, "f_lg", [128, 8], F32)
                lt = sb(e1, "f_lt", [128, 8], F32)
                mk1 = sb(e1, "f_mk1", [128, 8], F32)
                mk2 = sb(e1, "f_mk2", [128, 8], F32)
                mx = sb(e1, "f_mx", [128, 4], F32)
                st = sb(e1, "f_st", [128, 8], F32)
                wr = sb(e1, "f_wr", [128, 8, 8], F32)
                pbank = [ps(e1, f"f_p{i}", [128, 512]) for i in range(8)]
                if moe:
                    K.dma(SP, wr[:], w_router[li].rearrange("(k p) e -> p k e", p=128), writes=[wr])
                for sbk in range(2):
                    for b4 in range(4):
                        blk = sbk * 4 + b4
                        for j in range(4):
                            t = blk * 4 + j
                            xx = xt[j % 2]
                            K.dma(SP, xx[:], X1[t * 128:(t + 1) * 128, :], reads=[X1b], writes=[xx])
                            for k in range(8):
                                tr(pbank[k][:, j * 128:(j + 1) * 128], xx[:, k * 128:(k + 1) * 128], cst[:, C_ID:C_ID + 128], [xx, cst], [pbank[k]])
                        for k in range(8):
                            act(hT[:, k, b4 * 512:(b4 + 1) * 512], pbank[k][:], AF.Identity, [pbank[k], modcol], [hT],
                                scale=modcol[:, 24 + k:25 + k], bias=modcol[:, 16 + k:17 + k])
                        if moe:
                            h32 = wst[:, 0:2, :].rearrange("p a (k t) -> p (a k) t", t=512)
                            for k in range(8):
                                act(h32[:, k, :], pbank[k][:], AF.Identity, [pbank[k], modcol], [wst],
                                    scale=modcol[:, 24 + k:25 + k], bias=modcol[:, 16 + k:17 + k])
                            for j in range(4):
                                tl = b4 * 4 + j
                                pr = pbank[j]
                                for k in range(8):
                                    mm(pr[:, 0:8], h32[:, k, j * 128:(j + 1) * 128], wr[:, k, :], k == 0, k == 7, [wst, wr], [pr])
                                cp(DVE, lg[:], pr[:, 0:8], [pr], [lg])
                                K.op(DVE, lambda: nc.vector.tensor_reduce(out=mx[:, 0:1], in_=lg[:], axis=AX.X, op=ALU.max), [lg], [mx])
                                ts(DVE, mk1[:], lg[:], mx[:, 0:1], None, ALU.is_ge, None, [lg, mx], [mk1])
                                stt(lt[:], mk1[:], -1e30, lg[:], ALU.mult, ALU.add, [mk1, lg], [lt])
                                K.op(DVE, lambda: nc.vector.tensor_reduce(out=mx[:, 1:2], in_=lt[:], axis=AX.X, op=ALU.max), [lt], [mx])
                                ts(DVE, mk2[:], lt[:], mx[:, 1:2], None, ALU.is_ge, None, [lt, mx], [mk2])
                                tt(DVE, mx[:, 2:3], mx[:, 1:2], mx[:, 0:1], ALU.subtract, [mx], [mx])
                                act(mx[:, 2:3], mx[:, 2:3], AF.Exp, [mx], [mx])
                                ts(DVE, mx[:, 3:4], mx[:, 2:3], 1.0, None, ALU.add, None, [mx], [mx])
                                recip(mx[:, 3:4], mx[:, 3:4], [mx], [mx])
                                tt(DVE, mx[:, 2:3], mx[:, 2:3], mx[:, 3:4], ALU.mult, [mx], [mx])
                                ts(DVE, mk1[:], mk1[:], mx[:, 3:4], None, ALU.mult, None, [mk1, mx], [mk1])
                                stt(comb[:, tl, :], mk2[:], mx[:, 2:3], mk1[:], ALU.mult, ALU.add, [mk2, mx, mk1], [comb])
                    gi = 0
                    for e in range(NE):
                        if moe:
                            W1, W3, W2 = w1m[li, e], w3m[li, e], w2m[li, e]
                        else:
                            W1, W3, W2 = w1d[li], w3d[li], w2d[li]
                        for g in range(NG):
                            f0 = g * 256
                            wbb = wb[gi % 2]
                            gTT = gT[gi % 2]
                            gi += 1
                            K.dma(SP, wst[:, 0, :].rearrange("p (k n) -> p k n", n=256), W1[:, f0:f0 + 256].rearrange("(k p) n -> p k n", p=128), writes=[wst])
                            K.dma(SP, wst[:, 1, :].rearrange("p (k n) -> p k n", n=256), W3[:, f0:f0 + 256].rearrange("(k p) n -> p k n", p=128), writes=[wst])
                            K.dma(SP, wst[:, 2, :].rearrange("p (k n) -> p k n", n=1024), W2[f0:f0 + 256, :].rearrange("(k p) n -> p k n", p=128), writes=[wst])
                            cp(POOL, wbb[:, 0, :], wst[:, 0, :], [wst], [wbb])
                            cp(ACT if False else POOL, wbb[:, 1, :], wst[:, 1, :], [wst], [wbb])
                            cp(DVE, wbb[:, 2, :], wst[:, 2, :], [wst], [wbb])
                            w1v = wbb[:, 0, :].rearrange("p (k n) -> p k n", n=256)
                            w3v = wbb[:, 1, :].rearrange("p (k n) -> p k n", n=256)
                            w2v = wbb[:, 2, :].rearrange("p (k n) -> p k n", n=1024)
                            bi = 0
                            for fc in range(2):
                                for b4 in range(4):
                                    pa = pbank[(bi % 2) * 2]
                                    pb = pbank[(bi % 2) * 2 + 1]
                                    sa_ = sa[bi % 2]
                                    bi += 1
                                    ts_ = slice(b4 * 512, (b4 + 1) * 512)
                                    for k in range(8):
                                        mm(pa[:], w1v[:, k, fc * 128:(fc + 1) * 128], hT[:, k, ts_], k == 0, k == 7, [wbb, hT], [pa])
                                    for k in range(8):
                                        mm(pb[:], w3v[:, k, fc * 128:(fc + 1) * 128], hT[:, k, ts_], k == 0, k == 7, [wbb, hT], [pb])
                                    act(sa_[:], pa[:], AF.Silu, [pa], [sa_])
                                    tt(DVE, gTT[:, fc, ts_], pb[:], sa_[:], ALU.mult, [pb, sa_], [gTT])
                            for tl in range(16):
                                for hf in range(2):
                                    pp = pbank[4 + ((tl * 2 + hf) % 4)]
                                    for fc in range(2):
                                        mm(pp[:], gTT[:, fc, tl * 128:(tl + 1) * 128], w2v[:, fc, hf * 512:(hf + 1) * 512], fc == 0, fc == 1, [gTT, wbb], [pp])
                                    ya = yacc[:, tl, hf * 512:(hf + 1) * 512]
                                    first = (e == 0 and g == 0)
                                    if moe:
                                        if first:
                                            ts(DVE, ya, pp[:], comb[:, tl, e:e + 1], None, ALU.mult, None, [pp, comb], [yacc])
                                        else:
                                            stt(ya, pp[:], comb[:, tl, e:e + 1], ya, ALU.mult, ALU.add, [pp, comb, yacc], [yacc])
                                    else:
                                        if first:
                                            cp(DVE, ya, pp[:], [pp], [yacc])
                                        else:
                                            tt(DVE, ya, pp[:], ya, ALU.add, [pp, yacc], [yacc])
                    for tl in range(16):
                        t = sbk * 16 + tl
                        xx = xt[tl % 2]
                        K.dma(SP, xx[:], X1[t * 128:(t + 1) * 128, :], reads=[X1b], writes=[xx])
                        yv_ = yacc[:, tl, :]
                        tt(POOL, yv_, yv_, g2p_bc[:], ALU.mult, [yacc, g2p_bc], [yacc])
                        stt(xx[:], xx[:], ALPHA, yv_, ALU.mult, ALU.add, [xx, yacc], [xx])
                        layer_norm_store(xx, "ln2_g", "ln2_b", xdst, XSb, t, _TV(wst), st)
                K.barrier()
        K.barrier()
    return nc


class _TV(T):
    def __init__(self, wst):
        self.h = wst.h
        self.b = wst.b

    def __getitem__(self, idx):
        return self.h[:, 0, 0:1024]


def prep_inputs(inputs):
    f = lambda a: np.ascontiguousarray(np.asarray(a))
    w_in = f(inputs["w_in"])
    L = w_in.shape[0]
    h64 = np.arange(64)
    p64 = (h64 + 32) % 64
    p32 = (np.arange(32) + 16) % 32
    rq = np.arange(0, 256); rk = np.arange(256, 512); rv = np.arange(512, 768); rg = np.arange(768, 1024)
    dq = np.arange(1024, 1408); dk = np.arange(1408, 1792); dv = np.arange(1792, 2176)
    cq = np.arange(2176, 2560); ckv = np.arange(2560, 2816); kr = np.arange(2816, 2848)

    def permheads(cols, nh):
        return np.concatenate([cols[h * 64:(h + 1) * 64][p64] for h in range(nh)])
    fm_cols = np.concatenate([rq, rk, dq, dk, cq, ckv, kr])
    pm_cols = np.concatenate([permheads(rq, 4), permheads(rk, 4), permheads(dq, 6), permheads(dk, 6), kr[p32]])
    tm_cols = np.concatenate([rv, rg, dv])
    allc = np.concatenate([fm_cols, pm_cols, tm_cols])
    assert allc.shape[0] == WIN
    w_in_all = np.ascontiguousarray(w_in[:, :, allc])
    w_uq = f(inputs["w_uq"])
    uq_pm = np.concatenate([96 * h + 64 + p32 for h in range(6)])
    w_uq_all = np.ascontiguousarray(np.concatenate([w_uq, w_uq[:, :, uq_pm]], axis=2))
    w_ukv = f(inputs["w_ukv"])
    kn_cols = np.concatenate([128 * h + np.arange(64) for h in range(6)])
    v_cols = np.concatenate([128 * h + 64 + np.arange(64) for h in range(6)])
    w_ukv_r = np.ascontiguousarray(w_ukv[:, :, np.concatenate([kn_cols, v_cols])])
    shared = {
        "consts": host_consts(),
        "w_in_all": w_in_all,
        "ret_gn_g": f(inputs["ret_gn_g"]),
        "qn_gT": np.ascontiguousarray(f(inputs["mla_qn_g"]).reshape(L, 3, 128).transpose(0, 2, 1)),
        "kvn_gT": np.ascontiguousarray(f(inputs["mla_kvn_g"]).reshape(L, 2, 128).transpose(0, 2, 1)),
        "w_uq_all": w_uq_all,
        "w_ukv_r": w_ukv_r,
    }
    for k in ("w_out", "w_ada", "b_ada", "ln1_g", "ln1_b", "ln2_g", "ln2_b", "w1_dense", "w3_dense", "w2_dense",
              "w_router", "w1_moe", "w3_moe", "w2_moe"):
        shared[k] = f(inputs[k])
    x = f(inputs["x"]); c = f(inputs["c"]); pos = f(inputs["positions"]).astype(np.int32)
    maps = []
    for b in range(x.shape[0]):
        m = dict(shared)
        m["x"] = x[b]
        m["cT"] = np.ascontiguousarray(c[b].reshape(8, 128).T)
        m["pos"] = pos[b:b + 1]
        maps.append(m)
    return maps


def kernel(**inputs):
    maps = prep_inputs(inputs)
    nc = build()
    res = run_bass_kernel_spmd(nc, maps, core_ids=list(range(8)))
    return np.stack([np.asarray(r["out"]) for r in res.results], axis=0).astype(np.float32)
```

```python
import contextlib
import numpy as np
import concourse.bass as bass
import concourse.mybir as mybir
from concourse.bass_utils import run_bass_kernel_spmd

F32, BF16, I32 = mybir.dt.float32, mybir.dt.bfloat16, mybir.dt.int32
AF = mybir.ActivationFunctionType
ALU = mybir.AluOpType
AX = mybir.AxisListType

S = 4096
D = 1024
NT = 32
NB = 8
DEPTH = 4
ALPHA = (2.0 * DEPTH) ** 0.25
LN_EPS = 1e-5
RMS_EPS = 1e-6
THETA = 10000.0
NFM, NPM, NTM = 1952, 1312, 896
WIN = NFM + NPM + NTM
C_ID, C_LE, C_GE, C_DM, C_XI, C_ZE, C_IF64, C_SG64, C_IF32, C_SG32, C_SEL, C_END = (
    0, 128, 256, 384, 896, 1408, 1412, 1413, 1414, 1415, 1416, 1480)


def host_consts():
    c = np.zeros((128, C_END), np.float64)
    p = np.arange(128)
    c[:, C_ID:C_ID + 128] = np.eye(128)
    jj = p[:, None]
    ii = p[None, :]
    c[:, C_LE:C_LE + 128] = (jj <= ii)
    c[:, C_GE:C_GE + 128] = (jj >= ii)
    for h in range(4):
        lg = np.log(1.0 - 2.0 ** (-5.0 - h))
        c[:, C_DM + 128 * h:C_DM + 128 * (h + 1)] = np.where(ii >= jj, np.exp(np.maximum(ii - jj, 0) * lg), 0.0) * (64 ** -0.5)
        c[:, C_XI + 128 * h:C_XI + 128 * (h + 1)] = np.exp((ii + 1.0) * lg)
        c[:, C_ZE + h] = np.exp((127.0 - p) * lg) * (64 ** -0.5)
    c[:, C_IF64] = THETA ** (-(2.0 * (p % 32)) / 64.0) / (2 * np.pi)
    c[:, C_SG64] = np.where((p % 64) < 32, -2 * np.pi, 2 * np.pi)
    q = (p - 64) % 32
    c[:, C_IF32] = THETA ** (-(2.0 * (q % 16)) / 32.0) / (2 * np.pi)
    c[:, C_SG32] = np.where(q < 16, -2 * np.pi, 2 * np.pi)
    c[64, C_SEL:C_SEL + 64] = 1.0
    return c.astype(np.float32)


class Buf:
    __slots__ = ("w", "r", "name")

    def __init__(self, name=""):
        self.w = {}
        self.r = {}
        self.name = name


class T:
    def __init__(self, h, name):
        self.h = h
        self.b = Buf(name)

    def __getitem__(self, idx):
        return self.h[idx]


class Eng:
    def __init__(self, name, e, sem):
        self.name, self.e, self.sem, self.cnt, self.seen = name, e, sem, 0, {}
        self.ring = []
        self.di = 0

    def need(self, sem, val):
        if val <= 0 or self.seen.get(id(sem), 0) >= val:
            return
        self.e.wait_ge(sem, val)
        self.seen[id(sem)] = val


class Ctx:
    def __init__(self, nc, es):
        self.nc = nc
        self.es = es
        self.sems = {}
        mk = lambda n: es.enter_context(nc.semaphore(n))
        self.PE = Eng("pe", nc.tensor, mk("s_pe"))
        self.ACT = Eng("act", nc.scalar, mk("s_act"))
        self.DVE = Eng("dve", nc.vector, mk("s_dve"))
        self.POOL = Eng("pool", nc.gpsimd, mk("s_pool"))
        self.SP = Eng("sp", nc.sync, None)
        for i in range(8):
            self.SP.ring.append([mk(f"r_sp{i}"), 0])
            self.POOL.ring.append([mk(f"r_pl{i}"), 0])
        self.engs = [self.PE, self.ACT, self.DVE, self.POOL]
        self.nins = 0

    def _b(self, x):
        return x.b if isinstance(x, T) else x

    def _haz(self, E, reads, writes, own):
        for b in reads:
            for k, (sem, v) in b.w.items():
                E.need(sem, v)
        for b in writes:
            for k, (sem, v) in b.r.items():
                if sem is not own:
                    E.need(sem, v)
            for k, (sem, v) in b.w.items():
                if sem is not own:
                    E.need(sem, v)

    def _rec(self, sem, val, reads, writes):
        for b in reads:
            b.r[id(sem)] = (sem, val)
        for b in writes:
            if b.r:
                b.r = {}
                b.w = {}
            b.w[id(sem)] = (sem, val)

    def op(self, E, fn, reads=(), writes=()):
        reads = [self._b(x) for x in reads]
        writes = [self._b(x) for x in writes]
        self._haz(E, reads, writes, E.sem)
        ins = fn()
        E.cnt += 1
        ins.then_inc(E.sem, 1)
        self._rec(E.sem, E.cnt, reads, writes)
        self.nins += 1

    def dma(self, Q, out, in_, reads=(), writes=(), **kw):
        reads = [self._b(x) for x in reads]
        writes = [self._b(x) for x in writes]
        slot = Q.ring[Q.di % len(Q.ring)]
        Q.di += 1
        Q.need(slot[0], slot[1])
        self._haz(Q, reads, writes, None)
        ins = Q.e.dma_start(out=out, in_=in_, **kw)
        slot[1] += 16
        ins.then_inc(slot[0], 16)
        self._rec(slot[0], slot[1], reads, writes)
        self.nins += 1

    def barrier(self):
        tg = [(E.sem, E.cnt) for E in self.engs]
        for Q in (self.SP, self.POOL):
            tg += [(s[0], s[1]) for s in Q.ring]
        for E in self.engs + [self.SP]:
            for sem, v in tg:
                E.need(sem, v)


def build(nlayers=DEPTH, debug=False, stop=None):
    nc = bass.Bass("TRN2", target_bir_lowering=False)
    dt = nc.dram_tensor
    inp = lambda n, s, d=F32: dt(n, s, d, kind="ExternalInput").ap()
    skind = "ExternalOutput" if debug else "Internal"
    scr = lambda n, s, d=BF16: dt(n, s, d, kind=skind).ap()
    x_in = inp("x", [S, D])
    cT_in = inp("cT", [128, 8])
    pos_in = inp("pos", [1, S], I32)
    consts_in = inp("consts", [128, C_END])
    w_in_all = inp("w_in_all", [DEPTH, D, WIN])
    gn_g = inp("ret_gn_g", [DEPTH, 256])
    qn_gT = inp("qn_gT", [DEPTH, 128, 3])
    kvn_gT = inp("kvn_gT", [DEPTH, 128, 2])
    w_uq_all = inp("w_uq_all", [DEPTH, 384, 768])
    w_ukv_r = inp("w_ukv_r", [DEPTH, 256, 768])
    w_out = inp("w_out", [DEPTH, D, D])
    w_ada = inp("w_ada", [DEPTH, D, 6 * D])
    b_ada = inp("b_ada", [DEPTH, 6 * D])
    ln_in = {k: inp(k, [DEPTH, D]) for k in ("ln1_g", "ln1_b", "ln2_g", "ln2_b")}
    w1d = inp("w1_dense", [2, D, 2816])
    w3d = inp("w3_dense", [2, D, 2816])
    w2d = inp("w2_dense", [2, 2816, D])
    w_router = inp("w_router", [2, D, 8])
    w1m = inp("w1_moe", [2, 8, D, 3584])
    w3m = inp("w3_moe", [2, 8, D, 3584])
    w2m = inp("w2_moe", [2, 8, 3584, D])
    out = dt("out", [S, D], F32, kind="ExternalOutput").ap()
    XS = scr("XS", [S, D], F32)
    X1 = scr("X1", [S, D], F32)
    RQ = scr("RQ", [256, S])
    RQX = scr("RQX", [256, S])
    RK = scr("RK", [256, S])
    RV = scr("RV", [S, 256])
    RG = scr("RG", [S, 256])
    DQ = scr("DQ", [384, S])
    DK = scr("DK", [384, S])
    DV = scr("DV", [S, 384])
    MQ = scr("MQ", [6, 96, S])
    MKN = scr("MKN", [384, S])
    KR = scr("KR", [32, S])
    MV = scr("MV", [S, 384])
    CT = scr("CT", [16, 64, S])
    TB = scr("TB", [4, 128, S], F32)
    XSb, X1b = Buf("XS"), Buf("X1")
    SCR = Buf("scr_mix")
    CTb = Buf("CT")
    TBb = Buf("TB")

    with contextlib.ExitStack() as es:
        K = Ctx(nc, es)
        PE, ACT, DVE, POOL, SP = K.PE, K.ACT, K.DVE, K.POOL, K.SP

        uid = [0]

        def sb(es_, name, shape, dtype):
            uid[0] += 1
            name = f"{name}_{uid[0]}"
            return T(es_.enter_context(nc.sbuf_tensor(name, shape, dtype)), name)

        def ps(es_, name, shape, dtype=F32):
            uid[0] += 1
            name = f"{name}_{uid[0]}"
            return T(es_.enter_context(nc.psum_tensor(name, shape, dtype)), name)

        def act(o, i, func, reads, writes, **kw):
            K.op(ACT, lambda: nc.scalar.activation(out=o, in_=i, func=func, **kw), reads, writes)

        def tt(E, o, a, b, op, reads, writes):
            K.op(E, lambda: E.e.tensor_tensor(out=o, in0=a, in1=b, op=op), reads, writes)

        def ts(E, o, a, s1, s2, op0, op1, reads, writes):
            if s2 is None:
                K.op(E, lambda: E.e.tensor_scalar(out=o, in0=a, scalar1=s1, scalar2=None, op0=op0), reads, writes)
            else:
                K.op(E, lambda: E.e.tensor_scalar(out=o, in0=a, scalar1=s1, scalar2=s2, op0=op0, op1=op1), reads, writes)

        def stt(o, a, sc, b, op0, op1, reads, writes):
            K.op(DVE, lambda: nc.vector.scalar_tensor_tensor(out=o, in0=a, scalar=sc, in1=b, op0=op0, op1=op1), reads, writes)

        def cp(E, o, i, reads, writes):
            K.op(E, lambda: E.e.tensor_copy(out=o, in_=i), reads, writes)

        def mm(o, lhsT, rhs, start, stop, reads, writes):
            K.op(PE, lambda: nc.tensor.matmul(o, lhsT=lhsT, rhs=rhs, start=start, stop=stop), reads, writes)

        def tr(o, i, ident, reads, writes):
            K.op(PE, lambda: nc.tensor.transpose(o, i, ident), reads, writes)

        def recip(o, i, reads, writes):
            K.op(DVE, lambda: nc.vector.reciprocal(out=o, in_=i), reads, writes)

        def rsum(o, i, reads, writes):
            K.op(DVE, lambda: nc.vector.tensor_reduce(out=o, in_=i, axis=AX.X, op=ALU.add), reads, writes)

        cst = sb(es, "cst", [128, C_END], F32)
        K.dma(SP, cst[:], consts_in, writes=[cst])
        ident_bf = sb(es, "ident_bf", [128, 128], BF16)
        band_bf = sb(es, "band_bf", [128, 256], BF16)
        ones_f = sb(es, "ones_f", [128, 128], F32)
        ones_bf = sb(es, "ones_bf", [128, 128], BF16)
        eps_ln = sb(es, "eps_ln", [128, 1], F32)
        eps_rms = sb(es, "eps_rms", [128, 1], F32)
        cp(DVE, ident_bf[:], cst[:, C_ID:C_ID + 128], [cst], [ident_bf])
        cp(DVE, band_bf[:], cst[:, C_LE:C_LE + 256], [cst], [band_bf])
        negband = sb(es, "negband", [128, 256], BF16)
        ts(DVE, negband[:], cst[:, C_LE:C_LE + 256], -1.0, 30000.0, ALU.add, ALU.mult, [cst], [negband])
        K.op(DVE, lambda: nc.vector.memset(ones_f[:], 1.0), [], [ones_f])
        K.op(DVE, lambda: nc.vector.memset(ones_bf[:], 1.0), [], [ones_bf])
        K.op(DVE, lambda: nc.vector.memset(eps_ln[:], LN_EPS), [], [eps_ln])
        K.op(DVE, lambda: nc.vector.memset(eps_rms[:], RMS_EPS), [], [eps_rms])
        ident_f = cst
        silu_cT = sb(es, "silu_cT", [128, 8], F32)
        K.dma(SP, silu_cT[:], cT_in, writes=[silu_cT])
        act(silu_cT[:], silu_cT[:], AF.Silu, [silu_cT], [silu_cT])
        modcol = sb(es, "modcol", [128, 32], F32)
        g1p_bc = sb(es, "g1p_bc", [128, D], F32)
        g2p_bc = sb(es, "g2p_bc", [128, D], F32)
        lnp = {k: sb(es, "bc_" + k, [128, D], F32) for k in ln_in}

        with contextlib.ExitStack() as e1:
            posi = sb(e1, "posi", [128, S], I32)
            yv = sb(e1, "yv", [128, S], F32)
            ki = sb(e1, "ki", [128, S], I32)
            fr = sb(e1, "fr", [128, S], F32)
            m_ = sb(e1, "m_", [128, S], F32)
            K.dma(SP, posi[:], pos_in.broadcast_to([128, S]), writes=[posi])
            for ti, (cif, csg, shift) in enumerate(((C_IF64, None, 0.25), (C_IF64, C_SG64, 0.0), (C_IF32, None, 0.25), (C_IF32, C_SG32, 0.0))):
                cp(DVE, fr[:], posi[:], [posi], [fr])
                ts(DVE, yv[:], fr[:], cst[:, cif:cif + 1], shift, ALU.mult, ALU.add, [fr, cst], [yv])
                cp(DVE, ki[:], yv[:], [yv], [ki])
                cp(DVE, fr[:], ki[:], [ki], [fr])
                tt(DVE, fr[:], yv[:], fr[:], ALU.subtract, [yv, fr], [fr])
                ts(DVE, m_[:], fr[:], 0.5, None, ALU.is_gt, None, [fr], [m_])
                tt(DVE, fr[:], fr[:], m_[:], ALU.subtract, [fr, m_], [fr])
                ts(DVE, m_[:], fr[:], -0.5, None, ALU.is_lt, None, [fr], [m_])
                tt(DVE, fr[:], fr[:], m_[:], ALU.add, [fr, m_], [fr])
                if csg is None:
                    act(yv[:], fr[:], AF.Sin, [fr], [yv], scale=float(2 * np.pi))
                else:
                    act(yv[:], fr[:], AF.Sin, [fr, cst], [yv], scale=cst[:, csg:csg + 1])
                K.dma(SP, TB[ti], yv[:], reads=[yv], writes=[TBb])
            K.barrier()

        def load_hT(es_, xsrc, xbuf, blk, hT, col_sc, col_sh, xt, pst, hT32=None):
            for j in range(4):
                t = blk * 4 + j
                xx = xt[j % 2]
                K.dma(SP, xx[:], xsrc[t * 128:(t + 1) * 128, :], reads=[xbuf], writes=[xx])
                for k in range(8):
                    tr(pst[k][:, j * 128:(j + 1) * 128], xx[:, k * 128:(k + 1) * 128], cst[:, C_ID:C_ID + 128], [xx, cst], [pst[k]])
            for k in range(8):
                act(hT[:, k, :], pst[k][:], AF.Identity, [pst[k], modcol], [hT],
                    scale=modcol[:, col_sc + k:col_sc + k + 1], bias=modcol[:, col_sh + k:col_sh + k + 1])
                if hT32 is not None:
                    act(hT32[:, k, :], pst[k][:], AF.Identity, [pst[k], modcol], [hT32],
                        scale=modcol[:, col_sc + k:col_sc + k + 1], bias=modcol[:, col_sh + k:col_sh + k + 1])

        def layer_norm_store(u, gk, bk, dst, dstbuf, t, tmp, st):
            rsum(st[:, 0:1], u[:], [u], [st])
            act(tmp[:], u[:], AF.Square, [u], [tmp])
            rsum(st[:, 1:2], tmp[:], [tmp], [st])
            ts(DVE, st[:, 2:3], st[:, 0:1], 1.0 / D, None, ALU.mult, None, [st], [st])
            tt(DVE, st[:, 3:4], st[:, 2:3], st[:, 2:3], ALU.mult, [st], [st])
            stt(st[:, 4:5], st[:, 1:2], 1.0 / D, st[:, 3:4], ALU.mult, ALU.subtract, [st], [st])
            act(st[:, 5:6], st[:, 4:5], AF.Sqrt, [st, eps_ln], [st], bias=eps_ln[:, 0:1], scale=1.0)
            recip(st[:, 6:7], st[:, 5:6], [st], [st])
            stt(st[:, 7:8], st[:, 2:3], -1.0, st[:, 6:7], ALU.mult, ALU.mult, [st], [st])
            act(tmp[:], u[:], AF.Identity, [u, st], [tmp], scale=st[:, 6:7], bias=st[:, 7:8])
            tt(POOL, tmp[:], tmp[:], lnp[gk][:], ALU.mult, [tmp, lnp[gk]], [tmp])
            tt(POOL, u[:], tmp[:], lnp[bk][:], ALU.add, [tmp, lnp[bk]], [u])
            K.dma(SP, dst[t * 128:(t + 1) * 128, :], u[:], reads=[u], writes=[dstbuf])

        for l in range(nlayers):
            xsrc = x_in if l == 0 else XS
            xdst = out if l == nlayers - 1 else XS
            with contextlib.ExitStack() as e1:
                wad = [sb(e1, f"wad{i}", [128, 8, 512], F32) for i in range(2)]
                modrow = sb(e1, "modrow", [1, 6 * D], F32)
                brow = sb(e1, "brow", [1, 6 * D], F32)
                pm_ = [ps(e1, f"pm{i}", [128, 512]) for i in range(2)]
                pcol = ps(e1, "pcol", [128, 32])
                K.dma(SP, brow[:], b_ada[l:l + 1, :], writes=[brow])
                for k in lnp:
                    K.dma(SP, lnp[k][:], ln_in[k][l:l + 1, :].broadcast_to([128, D]), writes=[lnp[k]])
                for cb in range(12):
                    wv = wad[cb % 2]
                    K.dma(SP, wv[:], w_ada[l, :, cb * 512:(cb + 1) * 512].rearrange("(k p) n -> p k n", p=128), writes=[wv])
                    pp = pm_[cb % 2]
                    for k in range(8):
                        mm(pp[0:1, :], silu_cT[:, k:k + 1], wv[:, k, :], k == 0, k == 7, [silu_cT, wv], [pp])
                    tt(DVE, modrow[0:1, cb * 512:(cb + 1) * 512], pp[0:1, :], brow[0:1, cb * 512:(cb + 1) * 512], ALU.add,
                       [pp, brow], [modrow])
                for gi, (base, dst) in enumerate(((2 * D, g1p_bc), (5 * D, g2p_bc))):
                    for hf in range(2):
                        pp = pm_[hf]
                        mm(pp[:, :], ones_f[0:1, :], modrow[0:1, base + hf * 512: base + (hf + 1) * 512], True, True, [ones_f, modrow], [pp])
                        ts(DVE, dst[:, hf * 512:(hf + 1) * 512], pp[:], 1.0, None, ALU.add, None, [pp], [dst])
                for ci, base in enumerate((0, D, 3 * D, 4 * D)):
                    for k in range(8):
                        mm(pcol[:, ci * 8 + k: ci * 8 + k + 1], modrow[0:1, base + k * 128: base + (k + 1) * 128], ones_f[0:1, 0:1],
                           True, True, [modrow, ones_f], [pcol])
                cp(DVE, modcol[:], pcol[:], [pcol], [modcol])
                ts(DVE, modcol[:, 8:16], modcol[:, 8:16], 1.0, None, ALU.add, None, [modcol], [modcol])
                ts(DVE, modcol[:, 24:32], modcol[:, 24:32], 1.0, None, ALU.add, None, [modcol], [modcol])
                K.barrier()
            if stop == "M":
                break

            with contextlib.ExitStack() as e1:
                win = sb(e1, "win", [128, 8, WIN], BF16)
                for k in range(8):
                    K.dma(POOL, win[:, k, :], w_in_all[l, k * 128:(k + 1) * 128, :], writes=[win], max_dma_last_dim=4096)
                wuq = sb(e1, "wuq", [128, 3, 768], BF16)
                wukv = sb(e1, "wukv", [128, 2, 768], BF16)
                wq32 = sb(e1, "wq32", [128, 3, 768], F32)
                wkv32 = sb(e1, "wkv32", [128, 2, 768], F32)
                qng = sb(e1, "qng", [128, 3], F32)
                kvg = sb(e1, "kvg", [128, 2], F32)
                K.dma(SP, wq32[:], w_uq_all[l].rearrange("(k p) n -> p k n", p=128), writes=[wq32])
                K.dma(SP, wkv32[:], w_ukv_r[l].rearrange("(k p) n -> p k n", p=128), writes=[wkv32])
                K.dma(SP, qng[:], qn_gT[l], writes=[qng])
                K.dma(SP, kvg[:], kvn_gT[l], writes=[kvg])
                for k in range(3):
                    ts(DVE, wuq[:, k, :], wq32[:, k, :], qng[:, k:k + 1], None, ALU.mult, None, [wq32, qng], [wuq])
                for k in range(2):
                    ts(DVE, wukv[:, k, :], wkv32[:, k, :], kvg[:, k:k + 1], None, ALU.mult, None, [wkv32, kvg], [wukv])
                hT = sb(e1, "hT", [128, 8, 512], BF16)
                xt = [sb(e1, f"xt{i}", [128, D], F32) for i in range(2)]
                tb = sb(e1, "tb", [128, 4, 512], F32)
                fm = sb(e1, "fm", [128, 12, 512], BF16)
                tmo = sb(e1, "tmo", [128, 4, 896], BF16)
                t1 = sb(e1, "t1", [128, 512], F32)
                t2 = sb(e1, "t2", [128, 512], F32)
                cqT = sb(e1, "cqT", [128, 5, 512], BF16)
                sq = sb(e1, "sq", [128, 5, 512], F32)
                rs_q = sb(e1, "rs_q", [128, 512], F32)
                rs_kv = sb(e1, "rs_kv", [128, 512], F32)
                rs_col = sb(e1, "rs_col", [128, 4], F32)
                mqs = sb(e1, "mqs", [96, 6, 512], BF16)
                mkn = sb(e1, "mkn", [128, 3, 512], BF16)
                krs = sb(e1, "krs", [96, 512], BF16)
                mvs = sb(e1, "mvs", [128, 4, 384], BF16)
                with contextlib.ExitStack() as e2:
                    pst = [ps(e2, f"pst{i}", [128, 512]) for i in range(8)]
                    for blk in range(NB):
                        c0, c1 = blk * 512, (blk + 1) * 512
                        K.dma(SP, tb[:], TB[:, :, c0:c1].rearrange("a p t -> p a t"), reads=[TBb], writes=[tb])
                        load_hT(e2, xsrc, XSb, blk, hT, 8, 0, xt, pst)
                        bank = [0]

                        def nb():
                            bank[0] = (bank[0] + 1) % 8
                            return pst[bank[0]]
                        fm_off = [0, 128, 256, 384, 512, 640, 768, 896, 1024, 1152]
                        for ci in range(10):
                            pa, pb = nb(), nb()
                            fo = fm_off[ci]
                            for k in range(8):
                                mm(pa[:], win[:, k, fo:fo + 128], hT[:, k, :], k == 0, k == 7, [win, hT], [pa])
                            for k in range(8):
                                mm(pb[:], win[:, k, NFM + fo:NFM + fo + 128], hT[:, k, :], k == 0, k == 7, [win, hT], [pb])
                            tt(DVE, t1[:], pa[:], tb[:, 0, :], ALU.mult, [pa, tb], [t1])
                            tt(DVE, t2[:], pb[:], tb[:, 1, :], ALU.mult, [pb, tb], [t2])
                            slot = ci if ci < 2 else ci + 2
                            tt(DVE, fm[:, slot, :], t1[:], t2[:], ALU.add, [t1, t2], [fm])
                            if ci < 2:
                                for hh in range(2):
                                    h = ci * 2 + hh
                                    tt(POOL, fm[hh * 64:(hh + 1) * 64, 2 + ci, :].rearrange("p (c i) -> p c i", i=128),
                                       fm[hh * 64:(hh + 1) * 64, slot, :].rearrange("p (c i) -> p c i", i=128),
                                       cst[hh * 64:(hh + 1) * 64, C_XI + 128 * h:C_XI + 128 * (h + 1)].unsqueeze(1).broadcast_to([64, 4, 128]),
                                       ALU.mult, [fm, cst], [fm])
                        for ci in range(5):
                            pa = nb()
                            fo = 1280 + ci * 128
                            for k in range(8):
                                mm(pa[:], win[:, k, fo:fo + 128], hT[:, k, :], k == 0, k == 7, [win, hT], [pa])
                            act(cqT[:, ci, :], pa[:], AF.Copy, [pa], [cqT])
                            act(sq[:, ci, :], pa[:], AF.Square, [pa], [sq])
                        for (lo, n, rs, dim) in ((0, 3, rs_q, 384.0), (3, 2, rs_kv, 256.0)):
                            pa = nb()
                            for k in range(n):
                                mm(pa[:], ones_f[:], sq[:, lo + k, :], k == 0, k == n - 1, [ones_f, sq], [pa])
                            act(rs[:], pa[:], AF.Sqrt, [pa, eps_rms], [rs], scale=1.0 / dim, bias=eps_rms[:, 0:1])
                            recip(rs[:], rs[:], [rs], [rs])
                        pa, pb = nb(), nb()
                        for k in range(8):
                            mm(pa[64:96, :], win[:, k, 1920:1952], hT[:, k, :], k == 0, k == 7, [win, hT], [pa])
                        for k in range(8):
                            mm(pb[64:96, :], win[:, k, NFM + 1280:NFM + 1312], hT[:, k, :], k == 0, k == 7, [win, hT], [pb])
                        tt(DVE, t1[64:96, :], pa[64:96, :], tb[64:96, 2, :], ALU.mult, [pa, tb], [t1])
                        tt(DVE, t2[64:96, :], pb[64:96, :], tb[64:96, 3, :], ALU.mult, [pb, tb], [t2])
                        tt(DVE, krs[64:96, :], t1[64:96, :], t2[64:96, :], ALU.add, [t1, t2], [krs])
                        for h in range(6):
                            pa, pb = nb(), nb()
                            for k in range(3):
                                mm(pa[0:96, :], wuq[:, k, 96 * h:96 * h + 96], cqT[:, k, :], k == 0, k == 2, [wuq, cqT], [pa])
                            for k in range(3):
                                mm(pb[64:96, :], wuq[:, k, 576 + 32 * h:576 + 32 * h + 32], cqT[:, k, :], k == 0, k == 2, [wuq, cqT], [pb])
                            tt(DVE, mqs[0:64, h, :], pa[0:64, :], rs_q[0:64, :], ALU.mult, [pa, rs_q], [mqs])
                            tt(DVE, t1[64:96, :], pa[64:96, :], tb[64:96, 2, :], ALU.mult, [pa, tb], [t1])
                            tt(DVE, t2[64:96, :], pb[64:96, :], tb[64:96, 3, :], ALU.mult, [pb, tb], [t2])
                            tt(DVE, t1[64:96, :], t1[64:96, :], t2[64:96, :], ALU.add, [t1, t2], [t1])
                            tt(DVE, mqs[64:96, h, :], t1[64:96, :], rs_q[64:96, :], ALU.mult, [t1, rs_q], [mqs])
                        for c in range(3):
                            pa = nb()
                            for k in range(2):
                                mm(pa[:], wukv[:, k, c * 128:(c + 1) * 128], cqT[:, 3 + k, :], k == 0, k == 1, [wukv, cqT], [pa])
                            tt(DVE, mkn[:, c, :], pa[:], rs_kv[:], ALU.mult, [pa, rs_kv], [mkn])
                        for j in range(4):
                            tsl = slice(j * 128, (j + 1) * 128)
                            pa, pb, pc, pd = nb(), nb(), nb(), nb()
                            for k in range(8):
                                mm(pa[:], hT[:, k, tsl], win[:, k, NFM + NPM:NFM + NPM + 512], k == 0, k == 7, [win, hT], [pa])
                            for k in range(8):
                                mm(pb[:, 0:384], hT[:, k, tsl], win[:, k, NFM + NPM + 512:NFM + NPM + 896], k == 0, k == 7, [win, hT], [pb])
                            for k in range(2):
                                mm(pc[:, 0:384], cqT[:, 3 + k, tsl], wukv[:, k, 384:768], k == 0, k == 1, [wukv, cqT], [pc])
                            for k in range(2):
                                mm(pd[:, 0:1], sq[:, 3 + k, tsl], ones_f[:, 0:1], k == 0, k == 1, [sq, ones_f], [pd])
                            act(tmo[:, j, 0:256], pa[:, 0:256], AF.Copy, [pa], [tmo])
                            act(tmo[:, j, 256:512], pa[:, 256:512], AF.Silu, [pa], [tmo])
                            act(tmo[:, j, 512:896], pb[:, 0:384], AF.Copy, [pb], [tmo])
                            act(rs_col[:, j:j + 1], pd[:, 0:1], AF.Sqrt, [pd, eps_rms], [rs_col], scale=1.0 / 256.0, bias=eps_rms[:, 0:1])
                            recip(rs_col[:, j:j + 1], rs_col[:, j:j + 1], [rs_col], [rs_col])
                            ts(DVE, mvs[:, j, :], pc[:, 0:384], rs_col[:, j:j + 1], None, ALU.mult, None, [pc, rs_col], [mvs])
                        def fmv(dst, n):
                            return dst.rearrange("(c p) t -> p c t", p=128)[:, :, c0:c1]
                        K.dma(SP, fmv(RQ, 2), fm[:, 0:2, :], reads=[fm], writes=[SCR])
                        K.dma(SP, fmv(RQX, 2), fm[:, 2:4, :], reads=[fm], writes=[SCR])
                        K.dma(SP, fmv(RK, 2), fm[:, 4:6, :], reads=[fm], writes=[SCR])
                        K.dma(SP, fmv(DQ, 3), fm[:, 6:9, :], reads=[fm], writes=[SCR])
                        K.dma(SP, fmv(DK, 3), fm[:, 9:12, :], reads=[fm], writes=[SCR])
                        K.dma(SP, MQ[:, :, c0:c1].rearrange("h d t -> d h t"), mqs[:], reads=[mqs], writes=[SCR])
                        K.dma(SP, fmv(MKN, 3), mkn[:], reads=[mkn], writes=[SCR])
                        K.dma(SP, KR[:, c0:c1], krs[64:96, :], reads=[krs], writes=[SCR])
                        tmv = lambda dst: dst[c0:c1, :].rearrange("(j p) e -> p j e", p=128)
                        K.dma(SP, tmv(RV), tmo[:, :, 0:256], reads=[tmo], writes=[SCR])
                        K.dma(SP, tmv(RG), tmo[:, :, 256:512], reads=[tmo], writes=[SCR])
                        K.dma(SP, tmv(DV), tmo[:, :, 512:896], reads=[tmo], writes=[SCR])
                        K.dma(SP, tmv(MV), mvs[:], reads=[mvs], writes=[SCR])
                K.barrier()
            if stop == "A":
                break

            with contextlib.ExitStack() as e1:
                r_sets = [dict(qT=sb(e1, f"r_qT{i}", [64, S], BF16), qxT=sb(e1, f"r_qxT{i}", [64, S], BF16),
                               kT=sb(e1, f"r_kT{i}", [64, S], BF16), vv=sb(e1, f"r_v{i}", [128, NT, 64], BF16),
                               gg=sb(e1, f"r_g{i}", [128, NT, 64], BF16)) for i in range(2)]

                def r_load(h):
                    st_ = r_sets[h % 2]
                    hs_ = slice(h * 64, (h + 1) * 64)
                    K.dma(SP, st_["qT"][:], RQ[hs_, :], reads=[SCR], writes=[st_["qT"]])
                    K.dma(SP, st_["qxT"][:], RQX[hs_, :], reads=[SCR], writes=[st_["qxT"]])
                    K.dma(SP, st_["kT"][:], RK[hs_, :], reads=[SCR], writes=[st_["kT"]])
                    K.dma(SP, st_["vv"][:], RV[:, hs_].rearrange("(t p) e -> p t e", p=128), reads=[SCR], writes=[st_["vv"]])
                    K.dma(SP, st_["gg"][:], RG[:, hs_].rearrange("(t p) e -> p t e", p=128), reads=[SCR], writes=[st_["gg"]])
                kz = sb(e1, "r_kz", [128, NT, 64], BF16)
                prev = sb(e1, "r_prev", [64, NT, 64], BF16)
                state = sb(e1, "r_state", [64, 64], F32)
                sTm = [sb(e1, f"r_sTm{i}", [128, 4, 128], BF16) for i in range(2)]
                osb = sb(e1, "r_osb", [128, 8, 64], F32)
                osq = sb(e1, "r_osq", [128, 8, 64], F32)
                obf = sb(e1, "r_obf", [128, 8, 64], BF16)
                st = sb(e1, "r_st", [128, 6, 8], F32)
                gng = sb(e1, "r_gng", [128, 256], F32)
                ost = sb(e1, "r_ost", [64, 1024], BF16)
                ptr = ps(e1, "r_ptr", [128, 1024], BF16)
                pkv = [ps(e1, f"r_pkv{i}", [64, 512]) for i in range(1)]
                pss = [ps(e1, f"r_pss{i}", [128, 512]) for i in range(2)]
                pso = ps(e1, "r_pso", [128, 512])
                pot = ps(e1, "r_pot", [64, 1024], BF16)
                K.dma(SP, gng[:], gn_g[l:l + 1, :].broadcast_to([128, 256]), writes=[gng])
                r_load(0)
                for h in range(4):
                    lg = float(np.log(1.0 - 2.0 ** (-5.0 - h)))
                    gC = float(np.exp(128.0 * lg))
                    hs = slice(h * 64, (h + 1) * 64)
                    if h + 1 < 4:
                        r_load(h + 1)
                    qT, qxT, kT, vv, gg = (r_sets[h % 2][k_] for k_ in ("qT", "qxT", "kT", "vv", "gg"))
                    for g8 in range(4):
                        for c in range(8):
                            n = g8 * 8 + c
                            tr(ptr[:, c * 64:(c + 1) * 64], kT[:, n * 128:(n + 1) * 128], ident_bf[0:64, 0:64], [kT, ident_bf], [ptr])
                        act(kz[:, g8 * 8:(g8 + 1) * 8, :], ptr[:, 0:512].rearrange("p (c e) -> p c e", e=64), AF.Identity, [ptr, cst], [kz],
                            scale=cst[:, C_ZE + h:C_ZE + h + 1])
                    K.op(DVE, lambda: nc.vector.memset(state[:], 0.0), [], [state])
                    for g8 in range(4):
                        pk = pkv[0]
                        for c in range(8):
                            n = g8 * 8 + c
                            mm(pk[:, c * 64:(c + 1) * 64], kz[:, n, :], vv[:, n, :], True, True, [kz, vv], [pk])
                        for c in range(8):
                            n = g8 * 8 + c
                            cp(DVE, prev[:, n, :], state[:], [state], [prev])
                            stt(state[:], state[:], gC, pk[:, c * 64:(c + 1) * 64], ALU.mult, ALU.add, [state, pk], [state])
                    for g8 in range(4):
                        for half in range(2):
                            pS = pss[half]
                            for c in range(4):
                                n = g8 * 8 + half * 4 + c
                                cs = slice(n * 128, (n + 1) * 128)
                                mm(pS[:, c * 128:(c + 1) * 128], kT[:, cs], qT[:, cs], True, True, [kT, qT], [pS])
                            tt(DVE, sTm[half][:], pS[:].rearrange("p (c i) -> p c i", i=128),
                               cst[:, C_DM + 128 * h:C_DM + 128 * (h + 1)].unsqueeze(1).broadcast_to([128, 4, 128]),
                               ALU.mult, [pS, cst], [sTm[half]])
                            for c in range(4):
                                n = g8 * 8 + half * 4 + c
                                cc = half * 4 + c
                                cs = slice(n * 128, (n + 1) * 128)
                                mm(pso[:, cc * 64:(cc + 1) * 64], sTm[half][:, c, :], vv[:, n, :], True, False, [sTm[half], vv], [pso])
                                mm(pso[:, cc * 64:(cc + 1) * 64], qxT[:, cs], prev[:, n, :], False, True, [qxT, prev], [pso])
                        p3 = pso[:].rearrange("p (c e) -> p c e", e=64)
                        act(osb[:], p3, AF.Copy, [pso], [osb])
                        act(osq[:], p3, AF.Square, [pso], [osq])
                        rsum(st[:, 0, :], osb[:], [osb], [st])
                        rsum(st[:, 1, :], osq[:], [osq], [st])
                        ts(DVE, st[:, 2, :], st[:, 0, :], 1.0 / 64, None, ALU.mult, None, [st], [st])
                        tt(DVE, st[:, 3, :], st[:, 2, :], st[:, 2, :], ALU.mult, [st], [st])
                        stt(st[:, 4, :], st[:, 1, :], 1.0 / 64, st[:, 3, :], ALU.mult, ALU.subtract, [st], [st])
                        act(st[:, 5, :], st[:, 4, :], AF.Sqrt, [st, eps_ln], [st], bias=eps_ln[:, 0:1], scale=1.0)
                        recip(st[:, 5, :], st[:, 5, :], [st], [st])
                        tt(DVE, osb[:], osb[:], st[:, 2, :].unsqueeze(2).broadcast_to([128, 8, 64]), ALU.subtract, [osb, st], [osb])
                        tt(DVE, osb[:], osb[:], st[:, 5, :].unsqueeze(2).broadcast_to([128, 8, 64]), ALU.mult, [osb, st], [osb])
                        tt(DVE, osb[:], osb[:], gng[:, hs].unsqueeze(1).broadcast_to([128, 8, 64]), ALU.mult, [osb, gng], [osb])
                        tt(DVE, obf[:], osb[:], gg[:, g8 * 8:(g8 + 1) * 8, :], ALU.mult, [osb, gg], [obf])
                        for c in range(8):
                            tr(pot[:, c * 128:(c + 1) * 128], obf[:, c, :], ident_bf[:], [obf, ident_bf], [pot])
                        act(ost[:], pot[:], AF.Copy, [pot], [ost])
                        K.dma(SP, CT[h, :, g8 * 1024:(g8 + 1) * 1024], ost[:], reads=[ost], writes=[CTb])
                K.barrier()
            if stop == "R":
                break

            def normalize_store(e_, oacc, slot, nrm_ps, rcp, ostg):
                for qb in range(NB):
                    cs = slice(qb * 512, (qb + 1) * 512)
                    pn = nrm_ps[qb % 2]
                    mm(pn[0:64, :], cst[0:65, C_SEL:C_SEL + 64], oacc[0:65, cs], True, True, [cst, oacc], [pn])
                    recip(rcp[:, :], pn[0:64, :], [pn], [rcp])
                    tt(DVE, ostg[:, cs], oacc[0:64, cs], rcp[:, :], ALU.mult, [oacc, rcp], [ostg])
                K.dma(SP, CT[slot], ostg[:], reads=[ostg], writes=[CTb])

            with contextlib.ExitStack() as e1:
                d_sets = [dict(qT=sb(e1, f"d_qT{i}", [64, S], BF16), kT=sb(e1, f"d_kT{i}", [64, S], BF16),
                               vr=[sb(e1, f"d_v{i}_{j}", [128, NT, 65], BF16) for j in range(3)]) for i in range(2)]

                def d_load(h):
                    st_ = d_sets[h % 2]
                    hs_ = slice(h * 64, (h + 1) * 64)
                    K.dma(SP, st_["qT"][:], DQ[hs_, :], reads=[SCR], writes=[st_["qT"]])
                    K.dma(SP, st_["kT"][:], DK[hs_, :], reads=[SCR], writes=[st_["kT"]])
                    for pi_, r_ in enumerate((1, 4, 16)):
                        nkt_ = NT // r_
                        for rho_ in range(r_):
                            src = DV[:, hs_].rearrange("(kt j rr) e -> rr j kt e", j=128, rr=r_)[rho_]
                            K.dma(SP, st_["vr"][pi_][:, rho_ * nkt_:(rho_ + 1) * nkt_, 0:64], src, reads=[SCR], writes=[st_["vr"][pi_]])
                oacc = sb(e1, "d_oacc", [65, S], F32)
                rcp = sb(e1, "d_rcp", [64, 512], F32)
                ostg = sb(e1, "d_ostg", [64, S], BF16)
                pT = [sb(e1, f"d_pT{i}", [128, 256], BF16) for i in range(3)]
                pss = [ps(e1, f"d_pss{i}", [128, 512]) for i in range(3)]
                pso = [ps(e1, f"d_pso{i}", [128, 512]) for i in range(2)]
                nrm = [ps(e1, f"d_nrm{i}", [128, 512]) for i in range(2)]
                for st_ in d_sets:
                    for i in range(3):
                        K.op(POOL, lambda i=i, st_=st_: nc.gpsimd.memset(st_["vr"][i][:], 1.0), [], [st_["vr"][i]])
                it = 0
                d_load(0)
                for h in range(6):
                    hs = slice(h * 64, (h + 1) * 64)
                    if h + 1 < 6:
                        d_load(h + 1)
                    qT, kT, vr = d_sets[h % 2]["qT"], d_sets[h % 2]["kT"], d_sets[h % 2]["vr"]
                    for pi, r in enumerate((1, 4, 16)):
                        nkt = NT // r
                        L = S // r
                        qv = qT[:].rearrange("d (m rr) -> d rr m", rr=r)
                        kv_ = kT[:].rearrange("d (m rr) -> d rr m", rr=r)
                        ov = oacc[:].rearrange("d (m rr) -> d rr m", rr=r)
                        for rho in range(r):
                            for kt in range(nkt):
                                nq = 256 if kt < nkt - 1 else 128
                                pS = pss[it % 3]
                                pt = pT[it % 3]
                                it += 1
                                mm(pS[:, 0:nq], kv_[:, rho, kt * 128:(kt + 1) * 128], qv[:, rho, kt * 128:kt * 128 + nq], True, False, [kT, qT], [pS])
                                mm(pS[:, 0:nq], ident_bf[:], negband[:, 0:nq], False, True, [ident_bf, negband], [pS])
                                act(pt[:, 0:nq], pS[:, 0:nq], AF.Exp, [pS], [pt], scale=0.125)
                                bk = kt // 4
                                po = pso[bk % 2]
                                vt = vr[pi][:, rho * nkt + kt, :]
                                c = kt % 4
                                mm(po[0:65, c * 128:(c + 1) * 128], vt, pt[:, 0:128], kt == 0, True, [vr[pi], pt], [po])
                                if nq == 256:
                                    bk2 = (kt + 1) // 4
                                    po2 = pso[bk2 % 2]
                                    c2 = (kt + 1) % 4
                                    mm(po2[0:65, c2 * 128:(c2 + 1) * 128], vt, pt[:, 128:256], True, False, [vr[pi], pt], [po2])
                                if c == 3 or kt == nkt - 1:
                                    w = (c + 1) * 128
                                    m0 = bk * 512
                                    if pi == 0:
                                        act(ov[0:65, rho, m0:m0 + w], po[0:65, 0:w], AF.Copy, [po], [oacc])
                                    else:
                                        tt(DVE, ov[0:65, rho, m0:m0 + w], po[0:65, 0:w], ov[0:65, rho, m0:m0 + w], ALU.add, [po, oacc], [oacc])
                    normalize_store(e1, oacc, 4 + h, nrm, rcp, ostg)
                K.barrier()
            if stop == "D":
                break

            with contextlib.ExitStack() as e1:
                m_sets = [dict(qT=sb(e1, f"m_qT{i}", [96, S], BF16), kT=sb(e1, f"m_kT{i}", [96, S], BF16),
                               vt=sb(e1, f"m_v{i}", [128, NT, 65], BF16)) for i in range(2)]

                def m_load(h):
                    st_ = m_sets[h % 2]
                    K.dma(SP, st_["qT"][:], MQ[h], reads=[SCR], writes=[st_["qT"]])
                    K.dma(SP, st_["kT"][0:64, :], MKN[h * 64:(h + 1) * 64, :], reads=[SCR], writes=[st_["kT"]])
                    K.dma(SP, st_["kT"][64:96, :], KR, reads=[SCR], writes=[st_["kT"]])
                    K.dma(SP, st_["vt"][:, :, 0:64], MV[:, h * 64:(h + 1) * 64].rearrange("(t p) e -> p t e", p=128), reads=[SCR], writes=[st_["vt"]])
                oacc = sb(e1, "m_oacc", [65, S], F32)
                rcp = sb(e1, "m_rcp", [64, 512], F32)
                ostg = sb(e1, "m_ostg", [64, S], BF16)
                pT = [sb(e1, f"m_pT{i}", [128, 512], BF16) for i in range(3)]
                pss = [ps(e1, f"m_pss{i}", [128, 512]) for i in range(3)]
                pso = [ps(e1, f"m_pso{i}", [128, 512]) for i in range(2)]
                nrm = [ps(e1, f"m_nrm{i}", [128, 512]) for i in range(2)]
                for st_ in m_sets:
                    K.op(POOL, lambda st_=st_: nc.gpsimd.memset(st_["vt"][:], 1.0), [], [st_["vt"]])
                sc = float(96 ** -0.5)
                it = 0
                m_load(0)
                for h in range(6):
                    if h + 1 < 6:
                        m_load(h + 1)
                    qT, kT, vt_ = m_sets[h % 2]["qT"], m_sets[h % 2]["kT"], m_sets[h % 2]["vt"]
                    for qb in range(NB):
                        po = pso[qb % 2]
                        nk = 4 * qb + 4
                        for kt in range(nk):
                            j = kt - 4 * qb
                            q0 = 0 if j < 0 else 128 * j
                            pS = pss[it % 3]
                            pt = pT[it % 3]
                            it += 1
                            mm(pS[:, q0:512], kT[:, kt * 128:(kt + 1) * 128], qT[:, qb * 512 + q0:(qb + 1) * 512], True, j < 0, [kT, qT], [pS])
                            if j >= 0:
                                mm(pS[:, q0:q0 + 128], ident_bf[:], negband[:, 0:128], False, True, [ident_bf, negband], [pS])
                            act(pt[:, q0:512], pS[:, q0:512], AF.Exp, [pS], [pt], scale=sc)
                            mm(po[0:65, q0:512], vt_[:, kt, :], pt[:, q0:512], kt == 0, kt == nk - 1, [vt_, pt], [po])
                        act(oacc[0:65, qb * 512:(qb + 1) * 512], po[0:65, :], AF.Copy, [po], [oacc])
                    normalize_store(e1, oacc, 10 + h, nrm, rcp, ostg)
                K.barrier()
            if stop == "ML":
                break

            with contextlib.ExitStack() as e1:
                wo32 = sb(e1, "wo32", [128, 8, D], F32)
                wo = sb(e1, "wo", [128, 8, D], BF16)
                K.dma(SP, wo32[:], w_out[l].rearrange("(c p) n -> p c n", p=128), writes=[wo32])
                for c in range(8):
                    tt(POOL if c % 2 else DVE, wo[:, c, :], wo32[:, c, :], g1p_bc[:], ALU.mult, [wo32, g1p_bc], [wo])
                ct = [sb(e1, f"o_ct{i}", [128, 8, 512], BF16) for i in range(2)]
                xt = [sb(e1, f"o_xt{i}", [128, D], F32) for i in range(2)]
                u = [sb(e1, f"o_u{i}", [128, D], F32) for i in range(2)]
                tmp = [sb(e1, f"o_tmp{i}", [128, D], F32) for i in range(2)]
                st = [sb(e1, f"o_st{i}", [128, 8], F32) for i in range(2)]
                py = [ps(e1, f"o_py{i}", [128, 512]) for i in range(4)]
                for blk in range(NB):
                    cb = ct[blk % 2]
                    K.dma(SP, cb[:], CT[:, :, blk * 512:(blk + 1) * 512].rearrange("(c s) d t -> (s d) c t", s=2), reads=[CTb], writes=[cb])
                    for j in range(4):
                        t = blk * 4 + j
                        xx = xt[t % 2]
                        uu = u[t % 2]
                        K.dma(SP, xx[:], xsrc[t * 128:(t + 1) * 128, :], reads=[XSb], writes=[xx])
                        for hf in range(2):
                            pp = py[(t % 2) * 2 + hf]
                            for c in range(8):
                                mm(pp[:], cb[:, c, j * 128:(j + 1) * 128], wo[:, c, hf * 512:(hf + 1) * 512], c == 0, c == 7, [cb, wo], [pp])
                            stt(uu[:, hf * 512:(hf + 1) * 512], xx[:, hf * 512:(hf + 1) * 512], ALPHA, pp[:], ALU.mult, ALU.add, [xx, pp], [uu])
                        layer_norm_store(uu, "ln1_g", "ln1_b", X1, X1b, t, tmp[t % 2], st[t % 2])
                K.barrier()
            if stop == "O":
                break

            moe = (l % 2 == 1)
            li = l // 2
            NE = 8 if moe else 1
            DFF = 3584 if moe else 2816
            NG = DFF // 256
            with contextlib.ExitStack() as e1:
                hT = sb(e1, "f_hT", [128, 8, 2048], BF16)
                yacc = sb(e1, "f_yacc", [128, 16, D], F32)
                gT = [sb(e1, f"f_gT{i}", [128, 2, 2048], BF16) for i in range(2)]
                gTb = [[Buf(f"gTb{i}_{j}") for j in range(4)] for i in range(2)]
                wst = sb(e1, "f_wst", [128, 3, 2048], F32)
                wb = [sb(e1, f"f_wb{i}", [128, 3, 2048], BF16) for i in range(2)]
                xt = [sb(e1, f"f_xt{i}", [128, D], F32) for i in range(2)]
                sa = [sb(e1, f"f_sa{i}", [128, 512], BF16) for i in range(2)]
                comb = sb(e1, "f_comb", [128, 16, 8], F32)
                lg = sb(e1, "f_lg", [128, 8], F32)
                lt = sb(e1, "f_lt", [128, 8], F32)
                mk1 = sb(e1, "f_mk1", [128, 8], F32)
                mk2 = sb(e1, "f_mk2", [128, 8], F32)
                mx = sb(e1, "f_mx", [128, 4], F32)
                st = sb(e1, "f_st", [128, 8], F32)
                wr = sb(e1, "f_wr", [128, 8, 8], F32)
                pbank = [ps(e1, f"f_p{i}", [128, 512]) for i in range(8)]
                if moe:
                    K.dma(SP, wr[:], w_router[li].rearrange("(k p) e -> p k e", p=128), writes=[wr])
                for sbk in range(2):
                    for b4 in range(4):
                        blk = sbk * 4 + b4
                        for j in range(4):
                            t = blk * 4 + j
                            xx = xt[j % 2]
                            K.dma(SP, xx[:], X1[t * 128:(t + 1) * 128, :], reads=[X1b], writes=[xx])
                            for k in range(8):
                                tr(pbank[k][:, j * 128:(j + 1) * 128], xx[:, k * 128:(k + 1) * 128], cst[:, C_ID:C_ID + 128], [xx, cst], [pbank[k]])
                        for k in range(8):
                            act(hT[:, k, b4 * 512:(b4 + 1) * 512], pbank[k][:], AF.Identity, [pbank[k], modcol], [hT],
                                scale=modcol[:, 24 + k:25 + k], bias=modcol[:, 16 + k:17 + k])
                        if moe:
                            h32 = wst[:, 0:2, :].rearrange("p a (k t) -> p (a k) t", t=512)
                            for k in range(8):
                                act(h32[:, k, :], pbank[k][:], AF.Identity, [pbank[k], modcol], [wst],
                                    scale=modcol[:, 24 + k:25 + k], bias=modcol[:, 16 + k:17 + k])
                            for j in range(4):
                                tl = b4 * 4 + j
                                pr = pbank[j]
                                for k in range(8):
                                    mm(pr[:, 0:8], h32[:, k, j * 128:(j + 1) * 128], wr[:, k, :], k == 0, k == 7, [wst, wr], [pr])
                                cp(DVE, lg[:], pr[:, 0:8], [pr], [lg])
                                K.op(DVE, lambda: nc.vector.tensor_reduce(out=mx[:, 0:1], in_=lg[:], axis=AX.X, op=ALU.max), [lg], [mx])
                                ts(DVE, mk1[:], lg[:], mx[:, 0:1], None, ALU.is_ge, None, [lg, mx], [mk1])
                                stt(lt[:], mk1[:], -1e30, lg[:], ALU.mult, ALU.add, [mk1, lg], [lt])
                                K.op(DVE, lambda: nc.vector.tensor_reduce(out=mx[:, 1:2], in_=lt[:], axis=AX.X, op=ALU.max), [lt], [mx])
                                ts(DVE, mk2[:], lt[:], mx[:, 1:2], None, ALU.is_ge, None, [lt, mx], [mk2])
                                tt(DVE, mx[:, 2:3], mx[:, 1:2], mx[:, 0:1], ALU.subtract, [mx], [mx])
                                act(mx[:, 2:3], mx[:, 2:3], AF.Exp, [mx], [mx])
                                ts(DVE, mx[:, 3:4], mx[:, 2:3], 1.0, None, ALU.add, None, [mx], [mx])
                                recip(mx[:, 3:4], mx[:, 3:4], [mx], [mx])
                                tt(DVE, mx[:, 2:3], mx[:, 2:3], mx[:, 3:4], ALU.mult, [mx], [mx])
                                ts(DVE, mk1[:], mk1[:], mx[:, 3:4], None, ALU.mult, None, [mk1, mx], [mk1])
                                stt(comb[:, tl, :], mk2[:], mx[:, 2:3], mk1[:], ALU.mult, ALU.add, [mk2, mx, mk1], [comb])
                    gi = 0
                    for e in range(NE):
                        if moe:
                            W1, W3, W2 = w1m[li, e], w3m[li, e], w2m[li, e]
                        else:
                            W1, W3, W2 = w1d[li], w3d[li], w2d[li]
                        for g in range(NG):
                            f0 = g * 256
                            wbb = wb[gi % 2]
                            gTT = gT[gi % 2]
                            gi += 1
                            K.dma(SP, wst[:, 0, :].rearrange("p (k n) -> p k n", n=256), W1[:, f0:f0 + 256].rearrange("(k p) n -> p k n", p=128), writes=[wst])
                            K.dma(SP, wst[:, 1, :].rearrange("p (k n) -> p k n", n=256), W3[:, f0:f0 + 256].rearrange("(k p) n -> p k n", p=128), writes=[wst])
                            K.dma(SP, wst[:, 2, :].rearrange("p (k n) -> p k n", n=1024), W2[f0:f0 + 256, :].rearrange("(k p) n -> p k n", p=128), writes=[wst])
                            cp(POOL, wbb[:, 0, :], wst[:, 0, :], [wst], [wbb])
                            cp(ACT if False else POOL, wbb[:, 1, :], wst[:, 1, :], [wst], [wbb])
                            cp(DVE, wbb[:, 2, :], wst[:, 2, :], [wst], [wbb])
                            w1v = wbb[:, 0, :].rearrange("p (k n) -> p k n", n=256)
                            w3v = wbb[:, 1, :].rearrange("p (k n) -> p k n", n=256)
                            w2v = wbb[:, 2, :].rearrange("p (k n) -> p k n", n=1024)
                            bi = 0
                            gbs = gTb[(gi - 1) % 2]
                            first = (e == 0 and g == 0)
                            for b4 in range(4):
                                ts_ = slice(b4 * 512, (b4 + 1) * 512)
                                for fc in range(2):
                                    pa = pbank[(bi % 2) * 2]
                                    pb = pbank[(bi % 2) * 2 + 1]
                                    sa_ = sa[bi % 2]
                                    bi += 1
                                    for k in range(8):
                                        mm(pa[:], w1v[:, k, fc * 128:(fc + 1) * 128], hT[:, k, ts_], k == 0, k == 7, [wbb, hT], [pa])
                                    for k in range(8):
                                        mm(pb[:], w3v[:, k, fc * 128:(fc + 1) * 128], hT[:, k, ts_], k == 0, k == 7, [wbb, hT], [pb])
                                    act(sa_[:], pa[:], AF.Silu, [pa], [sa_])
                                    tt(DVE, gTT[:, fc, ts_], pb[:], sa_[:], ALU.mult, [pb, sa_], [gbs[b4]])
                                for tl in range(b4 * 4, b4 * 4 + 4):
                                    for hf in range(2):
                                        pp = pbank[4 + ((tl * 2 + hf) % 4)]
                                        for fc in range(2):
                                            mm(pp[:], gTT[:, fc, tl * 128:(tl + 1) * 128], w2v[:, fc, hf * 512:(hf + 1) * 512], fc == 0, fc == 1, [gbs[b4], wbb], [pp])
                                        ya = yacc[:, tl, hf * 512:(hf + 1) * 512]
                                        if moe:
                                            if first:
                                                ts(DVE, ya, pp[:], comb[:, tl, e:e + 1], None, ALU.mult, None, [pp, comb], [yacc])
                                            else:
                                                stt(ya, pp[:], comb[:, tl, e:e + 1], ya, ALU.mult, ALU.add, [pp, comb, yacc], [yacc])
                                        else:
                                            if first:
                                                cp(DVE, ya, pp[:], [pp], [yacc])
                                            else:
                                                tt(DVE, ya, pp[:], ya, ALU.add, [pp, yacc], [yacc])
                    for tl in range(16):
                        t = sbk * 16 + tl
                        xx = xt[tl % 2]
                        K.dma(SP, xx[:], X1[t * 128:(t + 1) * 128, :], reads=[X1b], writes=[xx])
                        yv_ = yacc[:, tl, :]
                        tt(POOL, yv_, yv_, g2p_bc[:], ALU.mult, [yacc, g2p_bc], [yacc])
                        stt(xx[:], xx[:], ALPHA, yv_, ALU.mult, ALU.add, [xx, yacc], [xx])
                        layer_norm_store(xx, "ln2_g", "ln2_b", xdst, XSb, t, _TV(wst), st)
                K.barrier()
        K.barrier()
    return nc


class _TV(T):
    def __init__(self, wst):
        self.h = wst.h
        self.b = wst.b

    def __getitem__(self, idx):
        return self.h[:, 0, 0:1024]


def prep_inputs(inputs):
    f = lambda a: np.ascontiguousarray(np.asarray(a))
    w_in = f(inputs["w_in"])
    L = w_in.shape[0]
    h64 = np.arange(64)
    p64 = (h64 + 32) % 64
    p32 = (np.arange(32) + 16) % 32
    rq = np.arange(0, 256); rk = np.arange(256, 512); rv = np.arange(512, 768); rg = np.arange(768, 1024)
    dq = np.arange(1024, 1408); dk = np.arange(1408, 1792); dv = np.arange(1792, 2176)
    cq = np.arange(2176, 2560); ckv = np.arange(2560, 2816); kr = np.arange(2816, 2848)

    def permheads(cols, nh):
        return np.concatenate([cols[h * 64:(h + 1) * 64][p64] for h in range(nh)])
    fm_cols = np.concatenate([rq, rk, dq, dk, cq, ckv, kr])
    pm_cols = np.concatenate([permheads(rq, 4), permheads(rk, 4), permheads(dq, 6), permheads(dk, 6), kr[p32]])
    tm_cols = np.concatenate([rv, rg, dv])
    allc = np.concatenate([fm_cols, pm_cols, tm_cols])
    assert allc.shape[0] == WIN
    w_in_all = np.ascontiguousarray(w_in[:, :, allc])
    w_uq = f(inputs["w_uq"])
    uq_pm = np.concatenate([96 * h + 64 + p32 for h in range(6)])
    w_uq_all = np.ascontiguousarray(np.concatenate([w_uq, w_uq[:, :, uq_pm]], axis=2))
    w_ukv = f(inputs["w_ukv"])
    kn_cols = np.concatenate([128 * h + np.arange(64) for h in range(6)])
    v_cols = np.concatenate([128 * h + 64 + np.arange(64) for h in range(6)])
    w_ukv_r = np.ascontiguousarray(w_ukv[:, :, np.concatenate([kn_cols, v_cols])])
    shared = {
        "consts": host_consts(),
        "w_in_all": w_in_all,
        "ret_gn_g": f(inputs["ret_gn_g"]),
        "qn_gT": np.ascontiguousarray(f(inputs["mla_qn_g"]).reshape(L, 3, 128).transpose(0, 2, 1)),
        "kvn_gT": np.ascontiguousarray(f(inputs["mla_kvn_g"]).reshape(L, 2, 128).transpose(0, 2, 1)),
        "w_uq_all": w_uq_all,
        "w_ukv_r": w_ukv_r,
    }
    for k in ("w_out", "w_ada", "b_ada", "ln1_g", "ln1_b", "ln2_g", "ln2_b", "w1_dense", "w3_dense", "w2_dense",
              "w_router", "w1_moe", "w3_moe", "w2_moe"):
        shared[k] = f(inputs[k])
    x = f(inputs["x"]); c = f(inputs["c"]); pos = f(inputs["positions"]).astype(np.int32)
    maps = []
    for b in range(x.shape[0]):
        m = dict(shared)
        m["x"] = x[b]
        m["cT"] = np.ascontiguousarray(c[b].reshape(8, 128).T)
        m["pos"] = pos[b:b + 1]
        maps.append(m)
    return maps


def kernel(**inputs):
    maps = prep_inputs(inputs)
    nc = build()
    res = run_bass_kernel_spmd(nc, maps, core_ids=list(range(8)))
    return np.stack([np.asarray(r["out"]) for r in res.results], axis=0).astype(np.float32)
```
